# Optimizing a Trainium2 kernel written in Bass

```python
import math
import jax
import jax.numpy as jnp
from jax import lax
import numpy as np

D_MODEL = 1024
BATCH = 1
SEQ = 16384
DEPTH = 1

HEAD_DIM = 64
N_HEADS_FOX = 8
N_HEADS_MOBA = 8
FOX_WIDTH = N_HEADS_FOX * HEAD_DIM
MOBA_WIDTH = N_HEADS_MOBA * HEAD_DIM
MIX_WIDTH = FOX_WIDTH + MOBA_WIDTH
IN_WIDTH = 3 * FOX_WIDTH + 3 * MOBA_WIDTH + N_HEADS_FOX
Q_BLOCK = 128
MOBA_BLOCK = 256
MOBA_TOPK = 3
MOBA_Q_CHUNK = 64
NUM_BUCKETS = 32
MAX_DISTANCE = 128
N_EXPERTS = 32
TOP_K = 4
D_EXPERT = D_MODEL
SWIGLU_LIMIT = 7.0
SWIGLU_ALPHA = 1.702
EXPERT_ROW_BLOCK = 128
RMS_EPS = 1e-6

kernel_name = "hybrid_fox_moba_moe_block"


def rms_norm(x, g):
    x32 = x.astype(jnp.float32)
    y = x32 * lax.rsqrt(jnp.mean(x32 * x32, axis=-1, keepdims=True) + RMS_EPS)
    return (y * g.astype(jnp.float32)).astype(x.dtype)


def t5_bucket(dist):
    dist = jnp.maximum(dist, 0)
    max_exact = NUM_BUCKETS // 2
    d = jnp.maximum(dist, 1).astype(jnp.float32)
    large = max_exact + (jnp.log(d / max_exact) / math.log(MAX_DISTANCE / max_exact)
                         * (NUM_BUCKETS - max_exact)).astype(jnp.int32)
    large = jnp.minimum(large, NUM_BUCKETS - 1)
    return jnp.where(dist < max_exact, dist, large)


def fox_attention(q, k, v, log_f):
    B, S, H, Dh = q.shape
    scale = Dh ** -0.5
    cum = jnp.cumsum(log_f, axis=1)
    cum_h = cum.transpose(0, 2, 1)
    kh = k.transpose(0, 2, 1, 3)
    vh = v.transpose(0, 2, 1, 3)
    nq = S // Q_BLOCK
    qb = q.reshape(B, nq, Q_BLOCK, H, Dh).transpose(1, 0, 3, 2, 4)
    cb = cum.reshape(B, nq, Q_BLOCK, H).transpose(1, 0, 3, 2)
    key_pos = jnp.arange(S)

    def block(args):
        i, qi, ci = args
        s = jnp.einsum('bhqd,bhkd->bhqk', qi, kh).astype(jnp.float32) * scale
        s = s + ci[..., None] - cum_h[:, :, None, :]
        qpos = i * Q_BLOCK + jnp.arange(Q_BLOCK)
        s = jnp.where(key_pos[None, :] <= qpos[:, None], s, -jnp.inf)
        p = jax.nn.softmax(s, axis=-1)
        return jnp.einsum('bhqk,bhkd->bhqd', p.astype(vh.dtype), vh)

    out = lax.map(block, (jnp.arange(nq), qb, cb))
    return out.transpose(1, 0, 3, 2, 4).reshape(B, S, H, Dh)


def moba_attention(q, k, v, rel_bias):
    B, S, H, Dh = q.shape
    scale = Dh ** -0.5
    s_pad = -(-S // MOBA_BLOCK) * MOBA_BLOCK
    pad = ((0, 0), (0, s_pad - S), (0, 0), (0, 0))
    q, k, v = jnp.pad(q, pad), jnp.pad(k, pad), jnp.pad(v, pad)
    nb = s_pad // MOBA_BLOCK
    k_sel_n = min(MOBA_TOPK, nb)
    kb = k.reshape(B, nb, MOBA_BLOCK, H, Dh).transpose(0, 3, 1, 2, 4)
    vb = v.reshape(B, nb, MOBA_BLOCK, H, Dh).transpose(0, 3, 1, 2, 4)
    kmean = jnp.mean(kb.astype(jnp.float32), axis=3)
    nq = s_pad // MOBA_Q_CHUNK
    qc = q.reshape(B, nq, MOBA_Q_CHUNK, H, Dh).transpose(1, 0, 2, 3, 4)
    bi = jnp.arange(B)[:, None, None, None]
    hi = jnp.arange(H)[None, None, :, None]
    hi5 = jnp.arange(H)[None, None, :, None, None]
    blk_pos = jnp.arange(MOBA_BLOCK)

    def chunk(args):
        i, qi = args
        qpos = i * MOBA_Q_CHUNK + jnp.arange(MOBA_Q_CHUNK)
        own = (i * MOBA_Q_CHUNK) // MOBA_BLOCK
        gate = jnp.einsum('bqhd,bhnd->bqhn', qi.astype(jnp.float32), kmean)
        gate = jnp.where(jnp.arange(nb) < own, gate, -jnp.inf)
        _, idx = lax.top_k(gate, k_sel_n)
        valid = jnp.arange(k_sel_n) < own
        ksel = kb[bi, hi, idx]
        vsel = vb[bi, hi, idx]
        s_sel = jnp.einsum('bqhd,bqhjpd->bqhjp', qi, ksel).astype(jnp.float32) * scale
        kpos_sel = idx[..., None] * MOBA_BLOCK + blk_pos
        dist_sel = qpos[None, :, None, None, None] - kpos_sel
        s_sel = s_sel + rel_bias[t5_bucket(dist_sel), hi5].astype(jnp.float32)
        s_sel = jnp.where(valid[None, None, None, :, None], s_sel, -jnp.inf)
        k_own = lax.dynamic_index_in_dim(kb, own, axis=2, keepdims=False)
        v_own = lax.dynamic_index_in_dim(vb, own, axis=2, keepdims=False)
        s_own = jnp.einsum('bqhd,bhpd->bqhp', qi, k_own).astype(jnp.float32) * scale
        dist_own = qpos[:, None] - (own * MOBA_BLOCK + blk_pos)[None, :]
        bias_own = rel_bias[t5_bucket(dist_own)].transpose(0, 2, 1)
        s_own = s_own + bias_own[None].astype(jnp.float32)
        s_own = jnp.where((dist_own >= 0)[None, :, None, :], s_own, -jnp.inf)
        logits = jnp.concatenate(
            [s_sel.reshape(B, MOBA_Q_CHUNK, H, k_sel_n * MOBA_BLOCK), s_own], axis=-1)
        p = jax.nn.softmax(logits, axis=-1).astype(vb.dtype)
        p_sel = p[..., :k_sel_n * MOBA_BLOCK].reshape(B, MOBA_Q_CHUNK, H, k_sel_n, MOBA_BLOCK)
        p_own = p[..., k_sel_n * MOBA_BLOCK:]
        return (jnp.einsum('bqhjp,bqhjpd->bqhd', p_sel, vsel)
                + jnp.einsum('bqhp,bhpd->bqhd', p_own, v_own))

    out = lax.map(chunk, (jnp.arange(nq), qc))
    return out.transpose(1, 0, 2, 3, 4).reshape(B, s_pad, H, Dh)[:, :S]


def clamped_swiglu(hdn):
    x_glu = jnp.minimum(hdn[..., :D_EXPERT], SWIGLU_LIMIT)
    x_lin = jnp.clip(hdn[..., D_EXPERT:], -SWIGLU_LIMIT, SWIGLU_LIMIT)
    return x_glu * jax.nn.sigmoid(SWIGLU_ALPHA * x_glu) * (x_lin + 1)


def moe_ffn(h, w_router, b_router, w_gate_up, b_gate_up, w_down, b_down):
    B, S, D = h.shape
    T = B * S
    xt = h.reshape(T, D)
    logits = (xt @ w_router).astype(jnp.float32) + b_router.astype(jnp.float32)
    top_v, top_i = lax.top_k(logits, TOP_K)
    gates = jax.nn.softmax(top_v, axis=-1)
    A = T * TOP_K
    e_flat = top_i.reshape(A)
    tok_flat = jnp.arange(A) // TOP_K
    g_flat = gates.reshape(A)
    order = jnp.argsort(e_flat)
    e_sorted, tok_sorted, g_sorted = e_flat[order], tok_flat[order], g_flat[order]
    counts = jnp.bincount(e_flat, length=N_EXPERTS)
    padded = ((counts + EXPERT_ROW_BLOCK - 1) // EXPERT_ROW_BLOCK) * EXPERT_ROW_BLOCK
    starts = jnp.cumsum(counts) - counts
    pends = jnp.cumsum(padded)
    pstarts = pends - padded
    dest = pstarts[e_sorted] + jnp.arange(A) - starts[e_sorted]
    n_rows = A + N_EXPERTS * EXPERT_ROW_BLOCK
    n_blk = n_rows // EXPERT_ROW_BLOCK
    rows = jnp.zeros((n_rows, D), xt.dtype).at[dest].set(xt[tok_sorted])
    block_e = jnp.minimum(
        jnp.searchsorted(pends, jnp.arange(n_blk) * EXPERT_ROW_BLOCK, side='right'), N_EXPERTS - 1)

    def expert_block(args):
        xb, e = args
        hdn = xb @ w_gate_up[e] + b_gate_up[e]
        return clamped_swiglu(hdn) @ w_down[e] + b_down[e]

    y = lax.map(expert_block, (rows.reshape(n_blk, EXPERT_ROW_BLOCK, D), block_e)).reshape(n_rows, D)
    out = jnp.zeros((T, D), y.dtype).at[tok_sorted].add(y[dest] * g_sorted[:, None].astype(y.dtype))
    return out.reshape(B, S, D)


def setup_inputs(seed: int = 0) -> dict:
    key = jax.random.key(seed)
    ks = jax.random.split(key, 20)
    nrm = jax.random.normal
    f32 = jnp.float32
    return {
        "x": nrm(ks[0], (BATCH, SEQ, D_MODEL), f32),
        "c": nrm(ks[1], (BATCH, D_MODEL), f32),
        "w_ada": nrm(ks[2], (DEPTH, D_MODEL, 6 * D_MODEL), f32) * (0.5 * D_MODEL ** -0.5),
        "b_ada": nrm(ks[3], (DEPTH, 6 * D_MODEL), f32) * 0.01,
        "g_pre_mix": 1.0 + 0.05 * nrm(ks[4], (DEPTH, D_MODEL), f32),
        "g_post_mix": 1.0 + 0.05 * nrm(ks[5], (DEPTH, D_MODEL), f32),
        "w_in": nrm(ks[6], (DEPTH, D_MODEL, IN_WIDTH), f32) * D_MODEL ** -0.5,
        "b_forget": nrm(ks[7], (DEPTH, N_HEADS_FOX), f32) * 0.1,
        "rel_bias": nrm(ks[8], (NUM_BUCKETS, N_HEADS_MOBA), f32) * 0.5,
        "w_out": nrm(ks[9], (DEPTH, MIX_WIDTH, D_MODEL), f32) * MIX_WIDTH ** -0.5,
        "g_pre_ffn": 1.0 + 0.05 * nrm(ks[10], (DEPTH, D_MODEL), f32),
        "g_post_ffn": 1.0 + 0.05 * nrm(ks[11], (DEPTH, D_MODEL), f32),
        "w_router": nrm(ks[12], (DEPTH, D_MODEL, N_EXPERTS), f32) * D_MODEL ** -0.5,
        "b_router": nrm(ks[13], (DEPTH, N_EXPERTS), f32) * 0.01,
        "w_gate_up": nrm(ks[14], (DEPTH, N_EXPERTS, D_MODEL, 2 * D_EXPERT), f32) * D_MODEL ** -0.5,
        "b_gate_up": nrm(ks[15], (DEPTH, N_EXPERTS, 2 * D_EXPERT), f32) * 0.01,
        "w_down": nrm(ks[16], (DEPTH, N_EXPERTS, D_EXPERT, D_MODEL), f32) * D_EXPERT ** -0.5,
        "b_down": nrm(ks[17], (DEPTH, N_EXPERTS, D_MODEL), f32) * 0.01,
    }


def reference(x, c, w_ada, b_ada, g_pre_mix, g_post_mix, w_in, b_forget, rel_bias, w_out,
              g_pre_ffn, g_post_ffn, w_router, b_router, w_gate_up, b_gate_up, w_down, b_down):
    B, S, D = x.shape
    cond = jax.nn.silu(c)
    for l in range(DEPTH):
        mod = cond @ w_ada[l] + b_ada[l]
        sh_m, sc_m, gt_m, sh_f, sc_f, gt_f = jnp.split(mod, 6, axis=-1)
        h = rms_norm(x, g_pre_mix[l]) * (1 + sc_m[:, None, :]) + sh_m[:, None, :]
        proj = h @ w_in[l]
        o = np.cumsum([FOX_WIDTH] * 3 + [MOBA_WIDTH] * 3)
        q_a, k_a, v_a = proj[..., :o[0]], proj[..., o[0]:o[1]], proj[..., o[1]:o[2]]
        q_b, k_b, v_b = proj[..., o[2]:o[3]], proj[..., o[3]:o[4]], proj[..., o[4]:o[5]]
        f_logit = proj[..., o[5]:]
        log_f = jax.nn.log_sigmoid(f_logit.astype(jnp.float32) + b_forget[l].astype(jnp.float32))
        hs_a = (B, S, N_HEADS_FOX, HEAD_DIM)
        hs_b = (B, S, N_HEADS_MOBA, HEAD_DIM)
        y_a = fox_attention(q_a.reshape(hs_a), k_a.reshape(hs_a), v_a.reshape(hs_a), log_f)
        y_b = moba_attention(q_b.reshape(hs_b), k_b.reshape(hs_b), v_b.reshape(hs_b), rel_bias)
        mix = jnp.concatenate([y_a.reshape(B, S, FOX_WIDTH), y_b.reshape(B, S, MOBA_WIDTH)], axis=-1)
        x = x + gt_m[:, None, :] * rms_norm(mix @ w_out[l], g_post_mix[l])
        h = rms_norm(x, g_pre_ffn[l]) * (1 + sc_f[:, None, :]) + sh_f[:, None, :]
        y = moe_ffn(h, w_router[l], b_router[l], w_gate_up[l], b_gate_up[l], w_down[l], b_down[l])
        x = x + gt_f[:, None, :] * rms_norm(y, g_post_ffn[l])
    return x
```

```python
import numpy as np
import ml_dtypes
from contextlib import ExitStack
import concourse.bass as bass
import concourse.mybir as mybir
from concourse.bass_utils import run_bass_kernel_spmd

F32 = mybir.dt.float32
BF16 = mybir.dt.bfloat16
AF = mybir.ActivationFunctionType
ALU = mybir.AluOpType
AX = mybir.AxisListType
NEG = -30000.0
D = 1024
NCORES = 8


class Res:
    __slots__ = ("name", "w", "r", "x")

    def __init__(self, name="", x=False):
        self.name = name
        self.w = None
        self.r = {}
        self.x = x


class Sched:
    EPOCH = 12000
    NDMA = 8

    def __init__(self, nc, stack):
        self.nc = nc
        self.stack = stack
        self.names = ["pe", "act", "dve", "pool", "sp"]
        self.cnt = {k: 0 for k in self.names}
        self.sems = {}
        self.ops = {k: [] for k in self.names}
        self.waited = {k: {} for k in self.names}
        self.dma_i = {k: 0 for k in self.names}

    def sem(self, key):
        if key not in self.sems:
            self.sems[key] = self.stack.enter_context(
                self.nc.semaphore("s_" + "_".join(str(k) for k in key)))
        return self.sems[key]

    def _need(self, e, ev, waits):
        if ev is None:
            return
        key, val = ev
        if key[0] == "c" and key[1] == e and e == "pe":
            return
        if self.waited[e].get(key, 0) >= val:
            return
        self.waited[e][key] = val
        waits.append((key, val))

    def op(self, e, fn, reads=(), writes=(), dma=False):
        writes = list(writes) + [r for r in reads if r.x]
        reads = [r for r in reads if not r.x]
        waits = []
        for r in reads:
            self._need(e, r.w, waits)
        for r in writes:
            self._need(e, r.w, waits)
            for k, v in r.r.items():
                self._need(e, (k, v), waits)
        if dma:
            i = self.dma_i[e]
            self.dma_i[e] += 1
            key = ("d", e, i % self.NDMA)
            val = 16 * (i // self.NDMA + 1)
            if i >= self.NDMA:
                self._need(e, (key, val - 16), waits)
            inc = 16
        else:
            k = self.cnt[e]
            self.cnt[e] += 1
            key = ("c", e, k // self.EPOCH)
            val = k % self.EPOCH + 1
            inc = 1
        ev = (key, val)
        self.sem(key)
        for (k2, v2) in waits:
            self.sem(k2)
        self.ops[e].append((waits, fn, key, inc))
        for r in reads:
            r.r[key] = max(r.r.get(key, 0), val)
        for r in writes:
            r.w = ev
            r.r = {}
        return ev

    def flush(self, final=()):
        nc = self.nc
        ops = self.ops
        sems = self.sems
        fin = [r.w for r in final if r.w is not None]
        with nc.Block() as block:
            def mk(ename):
                def body(eng):
                    for (waits, fn, key, inc) in ops[ename]:
                        for (k2, v2) in waits:
                            eng.wait_ge(sems[k2], v2)
                        fn(eng).then_inc(sems[key], inc)
                    if ename == "sp":
                        for (k2, v2) in fin:
                            eng.wait_ge(sems[k2], v2)
                        for q in self.names:
                            n = self.dma_i[q]
                            for j in range(min(n, self.NDMA)):
                                cntj = (n - 1 - j) // self.NDMA + 1
                                eng.wait_ge(sems[("d", q, j)], 16 * cntj)
                return body
            block.tensor(mk("pe"))
            block.scalar(mk("act"))
            block.vector(mk("dve"))
            block.gpsimd(mk("pool"))
            block.sync(mk("sp"))
        self.ops = {k: [] for k in self.names}


class Ring:
    def __init__(self, items, x=False):
        self.items = items
        self.res = [Res(x=x) for _ in items]
        self.i = 0

    def next(self):
        k = self.i % len(self.items)
        self.i += 1
        return self.items[k], self.res[k]


class K:
    def __init__(self, sch):
        self.s = sch

    def mm(self, out, lhsT, rhs, start=True, stop=True, R=(), W=()):
        self.s.op("pe", lambda e: e.matmul(out, lhsT=lhsT, rhs=rhs, start=start, stop=stop), R, W)

    def tr(self, out, in_, ident, R=(), W=()):
        self.s.op("pe", lambda e: e.transpose(out, in_, ident), R, W)

    def act(self, out, in_, func, bias=None, scale=None, accum=None, R=(), W=()):
        kw = {}
        if bias is not None:
            kw["bias"] = bias
        if scale is not None:
            kw["scale"] = scale
        if accum is not None:
            kw["accum_out"] = accum
        self.s.op("act", lambda e: e.activation(out=out, in_=in_, func=func, **kw), R, W)

    def ts(self, eng, out, in0, s1, s2, op0, op1=None, R=(), W=()):
        if op1 is None:
            self.s.op(eng, lambda e: e.tensor_scalar(out, in0, s1, None, op0), R, W)
        else:
            self.s.op(eng, lambda e: e.tensor_scalar(out, in0, s1, s2, op0, op1), R, W)

    def tt(self, eng, out, in0, in1, op, R=(), W=()):
        self.s.op(eng, lambda e: e.tensor_tensor(out, in0, in1, op), R, W)

    def stt(self, out, in0, scalar, in1, op0, op1, R=(), W=()):
        self.s.op("dve", lambda e: e.scalar_tensor_tensor(out, in0, scalar, in1, op0, op1), R, W)

    def cp(self, eng, out, in_, R=(), W=()):
        if eng == "act":
            self.s.op("act", lambda e: e.activation(out=out, in_=in_, func=AF.Copy), R, W)
        else:
            self.s.op(eng, lambda e: e.tensor_copy(out, in_), R, W)

    def memset(self, eng, out, val, R=(), W=()):
        self.s.op(eng, lambda e: e.memset(out, val), R, W)

    def recip(self, out, in_, R=(), W=()):
        self.s.op("dve", lambda e: e.reciprocal(out, in_), R, W)

    def dma(self, out, in_, R=(), W=(), q="sp", **kw):
        self.s.op(q, lambda e: e.dma_start(out=out, in_=in_, **kw), R, W, dma=True)


NE = 32


def build_fused(S, dbg=False):
    NT = S // 128
    NQC = S // 512
    NB = S // 256
    NBP = max(NB, 8)
    NOC = NQC // 8
    OC = [7 + 8 * j for j in range(NOC)]
    TPC = NOC * 512
    NTL = TPC // 128
    nc = bass.Bass("TRN2", target_bir_lowering=False)

    def din(name, shape, dt=F32):
        return nc.dram_tensor(name, shape, dt, kind="ExternalInput").ap()
    x = din("x", [S, D])
    win = din("win", [8, D, 385])
    wada = din("wadaA", [D, 2048])
    vecs = din("vecsA", [32, 128])
    bfg = din("bfg", [1, 8])
    mbias = din("mbias", [8, 128, 1024])
    cmask = din("cmask", [128, 1024])
    rb31 = din("rb31", [128, 8])
    onehot = din("onehot", [64, S], BF16)
    identf = din("identf", [128, 128])
    padk = din("padk", [128, NT])
    padG = din("padG", [128, 64])
    wout = din("wout", [8, 128, D])
    wadaB = din("wadaB", [D, 4096])
    vecsB = din("vecsB", [64, 128])
    wr_d = din("wr", [D, NE])
    br_d = din("br", [1, NE])
    wgu_d = din("wgu", [NE, D, 2 * D])
    bgu_d = din("bgu", [NE, 2 * D])
    wd_d = din("wd", [NE, D, D])
    bd_d = din("bd", [NE, D])
    out = nc.dram_tensor("out", [TPC, D], F32, kind="ExternalOutput").ap()
    mixd = nc.dram_tensor("mixd", [8, 128, TPC], BF16, kind=("ExternalOutput" if dbg else "Internal")).ap()
    X1d = nc.dram_tensor("X1d", [TPC, D], F32, kind="Internal").ap()

    def xrow(i):
        return 128 * (4 * OC[i // 4] + i % 4)

    with ExitStack() as st:
        sch = Sched(nc, st)
        k = K(sch)
        rMix = Res()
        KM = 9

        KVd = nc.dram_tensor("KVd", [8, 2, 128, S], BF16, kind="Internal").ap()
        Qd = nc.dram_tensor("Qd", [8, 2, 128, TPC], BF16, kind="Internal").ap()
        C3d = nc.dram_tensor("C3d", [8, 3, TPC], BF16, kind="Internal").ap()
        Cd8 = nc.dram_tensor("Cd8", [8, S], F32, kind="Internal").ap()
        NC2 = max(NQC, 2)
        Crd = nc.dram_tensor("Crd", [8, NC2], F32, kind="Internal").ap()
        rKVd = [[Res() for _ in range(2)] for _ in range(8)]
        rQd = [[Res() for _ in range(2)] for _ in range(8)]
        rC3d = Res(); rCd8 = Res(); rCrd = Res()

        sW = ExitStack()

        def sb(name, shape, dt=F32):
            return sW.enter_context(nc.sbuf_tensor("W_" + name, shape, dt))

        def pst(name, shape, dt=F32):
            return sW.enter_context(nc.psum_tensor("W_" + name, shape, dt))

        psT = Ring([pst("psT%d" % i, [128, 1024], BF16) for i in range(2)], x=True)
        psP = Ring([pst("psP%d" % i, [128, 512]) for i in range(5)], x=True)
        psO = Ring([pst("psO%d" % i, [128, 512]) for i in range(1)], x=True)

        Wall = sb("Wall", [128, 8, 8 * 385], BF16); rWall = Res()
        ball = sb("ball", [128, 64]); rball = Res()
        Wf32 = sb("Wf32", [128, 8, 8]); rWf32 = Res()
        Wfb = sb("Wfb", [128, 8, 8], BF16); rWfb = Res()
        idf = sb("idf", [128, 128]); ridf = Res()
        idb = sb("idb", [128, 128], BF16); ridb = Res()
        onesf = sb("onesf", [128, 512]); ronesf = Res()
        vT = sb("vT", [128, 32]); rvT = Res()
        vin = sb("vin", [32, 128]); rvin = Res()
        cond2 = sb("cond2", [128, 8, 2]); rcond = Res()
        sh2 = sb("sh2", [128, 8, 2]); rsh2 = Res()
        modT = sb("modT", [128, 32]); rmod = Res()
        gm = sb("gm", [128, 8]); rgm = Res()
        negfb8 = sb("negfb8", [8, 2]); rnegfb = Res()
        bf8 = sb("bf8", [8, 1]); rbf8 = Res()
        padGs = sb("padGs", [128, 64]); rpadG = Res()
        ss = sb("ss", [128, 4]); rss = Res()
        rstd = sb("rstd", [128, 4]); rrstd = Res()
        crefrow8 = sb("crefrow8", [8, NC2]); rcrefrow = Res()
        kmall = sb("kmall", [64, 8 * NBP]); rkmean = Res()
        km2 = sb("km2", [64, 2]); rkm2 = Res()
        W32_ring = Ring([sb("W32_%d" % i, [128, 8, 385]) for i in range(2)])
        wada_ring = Ring([sb("wa%d" % i, [128, 8, 512]) for i in range(1)])
        xt_ring = Ring([sb("xt%d" % i, [128, D]) for i in range(9)])
        junk = sb("junk", [128, D], BF16); rjunk = Res()
        xn_ring = Ring([sb("xn%d" % i, [128, D], BF16) for i in range(4)])
        xT_ring = Ring([sb("xT%d" % i, [128, 8, 512], BF16) for i in range(3)])
        kst_ring = Ring([sb("kst%d" % i, [128, 512], BF16) for i in range(6)])
        qst_ring = Ring([sb("qst%d" % i, [128, 512], BF16) for i in range(3)])
        q32_ring = Ring([sb("q32%d" % i, [64, 512]) for i in range(2)])
        fr_ring = Ring([sb("fr%d" % i, [8, 4, 512]) for i in range(1)])
        c_ring = Ring([sb("cr%d" % i, [8, 512]) for i in range(2)])
        c3_ring = Ring([sb("c3%d" % i, [8, 3, 512], BF16) for i in range(2)])
        G_ring = Ring([sb("G%d" % i, [128, 64]) for i in range(2)])
        mx_ring = Ring([sb("mx%d" % i, [128, 8]) for i in range(2)])
        MB_ring = Ring([sb("MB%d" % i, [128, 128], BF16) for i in range(2)])
        MBf_ring = Ring([sb("MBf%d" % i, [128, 128]) for i in range(2)])

        k.dma(idf[:], identf, W=[ridf])
        k.cp("dve", idb[:], idf[:], R=[ridf], W=[ridb])
        k.memset("pool", onesf[:], 1.0, W=[ronesf])
        k.dma(vin[:], vecs, W=[rvin])
        k.dma(bf8[:], bfg.rearrange("o h -> h o"), W=[rbf8])
        k.dma(padGs[:], padG, W=[rpadG])
        k.memset("dve", kmall[:], 0.0, W=[rkmean])
        k.memset("dve", crefrow8[:], 0.0, W=[rcrefrow])
        po, rpo = psO.next()
        k.tr(po[:, 0:32], vin[:], idf[0:32, 0:32], R=[rvin, ridf], W=[rpo])
        k.cp("dve", vT[:], po[:, 0:32], R=[rpo], W=[rvT])
        k.memset("pool", cond2[:], 0.0, W=[rcond])
        k.memset("pool", sh2[:], 0.0, W=[rsh2])
        k.act(cond2[:, :, 0], vT[:, 0:8], AF.Silu, R=[rvT], W=[rcond])
        po, rpo = psO.next()
        for blk in range(4):
            wa, rwa = wada_ring.next()
            k.dma(wa[:], wada[:, blk * 512:(blk + 1) * 512].rearrange("(k p) n -> p k n", p=128), W=[rwa])
            for jj in range(4):
                j = blk * 4 + jj
                for kk in range(8):
                    k.mm(po[:, 2 * j:2 * j + 2], wa[:, kk, jj * 128:(jj + 1) * 128], cond2[:, kk, :],
                         start=(kk == 0), stop=(kk == 7), R=[rwa, rcond], W=[rpo])
        k.cp("dve", modT[:], po[:, 0:32], R=[rpo], W=[rmod])
        modv = modT[:].rearrange("p (j t) -> p j t", t=2)
        k.tt("dve", sh2[:, :, 0], modv[:, 0:8, 0], vT[:, 16:24], ALU.add, R=[rmod, rvT], W=[rsh2])
        k.tt("dve", gm[:], modv[:, 8:16, 0], vT[:, 24:32], ALU.add, R=[rmod, rvT], W=[rgm])
        k.ts("dve", gm[:], gm[:], 1.0, None, ALU.add, R=[rgm], W=[rgm])
        k.tt("dve", gm[:], gm[:], vT[:, 8:16], ALU.mult, R=[rgm, rvT], W=[rgm])
        for hp in range(8):
            W32, rW32 = W32_ring.next()
            k.dma(W32[:], win[hp].rearrange("(k p) n -> p k n", p=128), W=[rW32])
            for kk in range(8):
                if kk % 2 == 0:
                    k.ts("dve", Wall[:, kk, hp * 385:(hp + 1) * 385], W32[:, kk, :], gm[:, kk:kk + 1], None, ALU.mult,
                         R=[rW32, rgm], W=[rWall])
                else:
                    k.act(Wall[:, kk, hp * 385:(hp + 1) * 385], W32[:, kk, :], AF.Identity, scale=gm[:, kk:kk + 1],
                          R=[rW32, rgm], W=[rWall])
            k.cp("dve", Wf32[:, :, hp], W32[:, :, 384], R=[rW32], W=[rWf32])
            po, rpo = psO.next()
            for i, (c0, m) in enumerate(((64, 128), (256, 128), (0, 64), (192, 64))):
                for kk in range(8):
                    k.mm(po[0:m, 2 * i:2 * i + 2], W32[:, kk, c0:c0 + m], sh2[:, kk, :],
                         start=(kk == 0), stop=(kk == 7), R=[rW32, rsh2], W=[rpo])
            k.cp("dve", ball[:, hp * 8:(hp + 1) * 8], po[:, 0:8], R=[rpo], W=[rball])
        for kk in range(8):
            k.ts("dve", Wfb[:, kk, :], Wf32[:, kk, :], gm[:, kk:kk + 1], None, ALU.mult, R=[rWf32, rgm], W=[rWfb])
        po, rpo = psO.next()
        for kk in range(8):
            k.mm(po[0:8, 0:2], Wf32[:, kk, :], sh2[:, kk, :], start=(kk == 0), stop=(kk == 7),
                 R=[rWf32, rsh2], W=[rpo])
        k.tt("dve", negfb8[:, 0:1], po[0:8, 0:1], bf8[:, 0:1], ALU.add, R=[rpo, rbf8], W=[rnegfb])
        k.ts("dve", negfb8[:, 0:1], negfb8[:, 0:1], -1.0, None, ALU.mult, R=[rnegfb], W=[rnegfb])

        def load_x(g_):
            lst = []
            for i in range(4):
                t = 4 * g_ + i
                xt, rxt = xt_ring.next()
                k.dma(xt[:], x[t * 128:(t + 1) * 128, :], W=[rxt])
                lst.append((xt, rxt))
            return lst

        def make_xT_a(xts):
            for i in range(4):
                xt, rxt = xts[i]
                k.act(junk[:], xt[:], AF.Square, accum=ss[:, i:i + 1], R=[rxt], W=[rjunk, rss])
            k.act(rstd[:], ss[:], AF.Sqrt, bias=1e-6, scale=1.0 / D, R=[rss], W=[rrstd])
            k.recip(rstd[:], rstd[:], R=[rrstd], W=[rrstd])
            xns = []
            for i in range(4):
                xt, rxt = xts[i]
                xn, rxn = xn_ring.next()
                k.ts("dve", xn[:], xt[:], rstd[:, i:i + 1], None, ALU.mult, R=[rxt, rrstd], W=[rxn])
                xns.append((xn, rxn))
            return xns

        def make_xT_b(xns):
            xT, rxT = xT_ring.next()
            for i in range(4):
                xn, rxn = xns[i]
                pt, rpt = psT.next()
                for kk in range(8):
                    k.tr(pt[:, kk * 128:(kk + 1) * 128], xn[:, kk * 128:(kk + 1) * 128], idb[:],
                         R=[rxn, ridb], W=[rpt])
                k.cp("dve", xT[:, :, i * 128:(i + 1) * 128], pt[:].rearrange("p (k t) -> p k t", k=8),
                     R=[rpt], W=[rxT])
            return xT, rxT

        tmplG = {}
        tmplMB = {}
        rtmpl = Res()
        tmf = sb("tmplMBf", [128, 128])
        for g_ in OC:
            for own in (2 * g_, 2 * g_ + 1):
                tg = sb("tmplG%d" % own, [128, 64])
                k.memset("pool", tg[:], -1e30, W=[rtmpl])
                if own > 0:
                    k.cp("dve", tg[:, 0:own], padGs[:, 0:own], R=[rpadG, rtmpl], W=[rtmpl])
                tmplG[own] = tg
                tb = sb("tmplMB%d" % own, [128, 128], BF16)
                k.memset("pool", tmf[:, 0:64], 0.0, W=[rtmpl])
                k.memset("pool", tmf[:, 64:128], NEG, W=[rtmpl])
                k.memset("pool", tmf[:, 64 + own:65 + own], 0.0, W=[rtmpl])
                k.cp("dve", tb[:], tmf[:], R=[rtmpl], W=[rtmpl])
                tmplMB[own] = tb

        prev_c = None
        cur_xT = make_xT_b(make_xT_a(load_x(0)))
        pend_x = load_x(1) if NQC > 1 else None
        pend_xn = None
        for g in range(NQC):
            xT, rxT = cur_xT
            cols = slice(g * 512, (g + 1) * 512)
            own_g = g in OC
            oj = OC.index(g) if own_g else -1
            qcols = slice(oj * 512, (oj + 1) * 512)
            pf, rpf = psP.next()
            for kk in range(8):
                k.mm(pf[0:8, :], Wfb[:, kk, :], xT[:, kk, :], start=(kk == 0), stop=(kk == 7), R=[rWfb, rxT], W=[rpf])
            fr, rfr = fr_ring.next()
            k.act(fr[:, 0, :], pf[0:8, :], AF.Exp, bias=negfb8[:, 0:1], scale=-1.0, R=[rpf, rnegfb], W=[rfr])
            k.act(fr[:, 1, :], fr[:, 0, :], AF.Ln, bias=1.0, scale=1.0, R=[rfr], W=[rfr])
            cr, rcr = c_ring.next()
            init = 0.0 if prev_c is None else prev_c[0][:, 511:512]
            rd = [rfr, ronesf] + ([] if prev_c is None else [prev_c[1]])
            k.s.op("dve", lambda e, cr=cr, fr=fr, init=init: e.tensor_tensor_scan(
                cr[:, :], onesf[0:8, :], fr[:, 1, :], init, ALU.mult, ALU.subtract), rd, [rcr])
            k.dma(Cd8[:, cols], cr[:, :], R=[rcr], W=[rCd8], q="pool")
            if prev_c is None:
                k.cp("dve", fr[:, 2, :], cr[:, :], R=[rcr], W=[rfr])
            else:
                k.ts("dve", fr[:, 2, :], cr[:, :], init, None, ALU.subtract, R=[rcr, prev_c[1]], W=[rfr])
                k.cp("dve", crefrow8[:, g:g + 1], init, R=[prev_c[1]], W=[rcrefrow])
            if own_g:
                c3, rc3 = c3_ring.next()
                k.cp("dve", c3[:, 0, :], fr[:, 2, :], R=[rfr], W=[rc3])
                k.tt("dve", fr[:, 3, :], fr[:, 2, :], c3[:, 0, :], ALU.subtract, R=[rfr, rc3], W=[rfr])
                k.cp("dve", c3[:, 1, :], fr[:, 3, :], R=[rfr], W=[rc3])
                k.tt("dve", fr[:, 2, :], fr[:, 3, :], c3[:, 1, :], ALU.subtract, R=[rfr, rc3], W=[rfr])
                k.cp("dve", c3[:, 2, :], fr[:, 2, :], R=[rfr], W=[rc3])
                for r3 in range(3):
                    k.dma(C3d[:, r3, qcols], c3[:, r3, :], R=[rc3], W=[rC3d], q="pool")
            prev_c = (cr, rcr)
            for hp in range(8):
                for kind in range(2):
                    w0 = hp * 385
                    ko = w0 + (64 if kind == 0 else 256)
                    qo = w0 + (0 if kind == 0 else 192)
                    bki = hp * 8 + (0 if kind == 0 else 2)
                    bqi = hp * 8 + (4 if kind == 0 else 6)
                    pk, rpk = psP.next()
                    for kk in range(8):
                        k.mm(pk[:, :], Wall[:, kk, ko:ko + 128], xT[:, kk, :], start=(kk == 0), stop=(kk == 7),
                             R=[rWall, rxT], W=[rpk])
                    kst, rkst = kst_ring.next()
                    k.act(kst[:, :], pk[:, :], AF.Identity, bias=ball[:, bki:bki + 1], R=[rpk, rball], W=[rkst])
                    if kind == 1:
                        for hb in range(2):
                            k.act(junk[0:64, 0:256], pk[0:64, hb * 256:(hb + 1) * 256], AF.Copy,
                                  accum=km2[:, hb:hb + 1], R=[rpk], W=[rjunk, rkm2])
                        for hb in range(2):
                            c_ = hp * NBP + 2 * g + hb
                            k.act(kmall[:, c_:c_ + 1], km2[:, hb:hb + 1], AF.Identity,
                                  bias=ball[0:64, bki:bki + 1], scale=1.0 / 256, R=[rkm2, rball], W=[rkmean])
                    k.dma(KVd[hp, kind, :, cols], kst[:], R=[rkst], W=[rKVd[hp][kind]])
                    if kind == 1 and g + 1 < NQC:
                        if hp == 0:
                            pend_xn = make_xT_a(pend_x)
                            pend_x = load_x(g + 2) if g + 2 < NQC else None
                        elif hp == 3:
                            cur_xT = make_xT_b(pend_xn)
                    if not own_g:
                        continue
                    pq, rpq = psP.next()
                    for kk in range(8):
                        k.mm(pq[0:64, :], Wall[:, kk, qo:qo + 64], xT[:, kk, :], start=(kk == 0), stop=(kk == 7),
                             R=[rWall, rxT], W=[rpq])
                    qst, rqst = qst_ring.next()
                    k.ts("dve", qst[0:64, :], pq[0:64, :], ball[0:64, bqi:bqi + 1], 0.125, ALU.add, ALU.mult,
                         R=[rpq, rball], W=[rqst])
                    if kind == 0:
                        k.dma(Qd[hp, kind, 0:64, qcols], qst[0:64, :], R=[rqst], W=[rQd[hp][kind]])
                        continue
                    q32, rq32 = q32_ring.next()
                    k.act(q32[:], pq[0:64, :], AF.Identity, bias=ball[0:64, bqi:bqi + 1], R=[rpq, rball], W=[rq32])
                    for i in range(4):
                        own = 2 * g + i // 2
                        MB, rMB = MB_ring.next()
                        k.cp("pool", MB[:], tmplMB[own][:], R=[rtmpl], W=[rMB])
                        if own > 0:
                            pg, rpg = psO.next()
                            k.mm(pg[:, 0:NBP], q32[:, i * 128:(i + 1) * 128], kmall[:, hp * NBP:(hp + 1) * NBP],
                                 R=[rq32, rkmean], W=[rpg])
                            G, rG = G_ring.next()
                            k.tt("dve", G[:, 0:NBP], pg[:, 0:NBP], tmplG[own][:, 0:NBP], ALU.add, R=[rpg, rtmpl], W=[rG])
                            mx, rmx = mx_ring.next()
                            k.s.op("dve", lambda e, mx=mx, G=G: e.max(mx[:], G[:, 0:NBP]), [rG], [rmx])
                            k.ts("dve", MB[:, 64:64 + own], G[:, 0:own], mx[:, 2:3], NEG, ALU.is_lt, ALU.mult,
                                 R=[rG, rmx], W=[rMB])
                        pt, rpt = psT.next()
                        k.tr(pt[:, 0:128], MB[:], idb[:], R=[rMB, ridb], W=[rpt])
                        k.cp("dve", qst[64:128, i * 128:(i + 1) * 128], pt[64:128, 0:128], R=[rpt], W=[rqst])
                    k.dma(Qd[hp, kind, :, qcols], qst[:], R=[rqst], W=[rQd[hp][kind]])
        k.dma(Crd[:, :], crefrow8[:, :], R=[rcrefrow], W=[rCrd], q="pool")
        sch.flush()
        sW.close()

        sA = ExitStack()

        def sb(name, shape, dt=F32):
            return sA.enter_context(nc.sbuf_tensor(name, shape, dt))

        def pst(name, shape, dt=F32):
            return sA.enter_context(nc.psum_tensor(name, shape, dt))

        psT = Ring([pst("psT%d" % i, [128, 1024], BF16) for i in range(2)], x=True)
        psP = Ring([pst("psP%d" % i, [128, 512]) for i in range(4)], x=True)
        psO = Ring([pst("psO%d" % i, [128, 512]) for i in range(2)], x=True)
        Kts = [sb("Kt%d" % i, [128, S], BF16) for i in range(2)]
        Vts = [sb("Vt%d" % i, [128, NT, 65], BF16) for i in range(2)]
        Qts = [sb("Qt%d" % i, [128, TPC], BF16) for i in range(2)]
        rKs = [Res(), Res()]; rVs = [Res(), Res()]; rQs = [Res(), Res()]
        idf = sb("idf", [128, 128]); ridf = Res()
        idb = sb("idb", [128, 128], BF16); ridb = Res()
        onesf = sb("onesf", [128, 512]); ronesf = Res()
        Mfox = sb("Mfox", [128, 1024], BF16); rMfox = Res()
        Mm_ring = Ring([sb("Mmoba%d" % i, [128, 1024], BF16) for i in range(2)])
        mtmp = sb("mtmp", [128, 1024]); rmtmp = Res()
        mt2_ring = Ring([sb("mtmp2_%d" % i, [128, 1024]) for i in range(1)])
        rb31s = sb("rb31s", [128, 8]); rrb31 = Res()
        padks = sb("padks", [128, NT]); rpadk = Res()
        kbn = padks; rkbn = rpadk
        kbf_ring = Ring([sb("kbf%d" % i, [128, NT]) for i in range(2)])
        crow_ring = Ring([sb("crow%d" % i, [1, NC2]) for i in range(2)])
        crefB_ring = Ring([sb("crefB%d" % i, [128, NC2]) for i in range(2)])
        negC_ring = Ring([sb("negC%d" % i, [128, NT]) for i in range(2)])
        cdt_ring = Ring([sb("cdt%d" % i, [NT, 128]) for i in range(2)])
        vts_ring = Ring([sb("vts%d" % i, [128, 512], BF16) for i in range(3)])
        P_ring = Ring([sb("P%d" % i, [128, 512], BF16) for i in range(4)])
        ot_ring = Ring([sb("ot%d" % i, [128, 512]) for i in range(2)])
        yt_ring = Ring([sb("yt%d" % i, [64, 512], BF16) for i in range(2)])
        rl_ring = Ring([sb("rl%d" % i, [128, 512]) for i in range(1)])
        bq_ring = Ring([sb("bq%d" % i, [128, NT]) for i in range(2)])

        k.dma(idf[:], identf, W=[ridf])
        k.cp("dve", idb[:], idf[:], R=[ridf], W=[ridb])
        k.memset("pool", onesf[:], 1.0, W=[ronesf])
        k.dma(rb31s[:], rb31, W=[rrb31])
        k.dma(padks[:], padk, W=[rpadk])
        k.dma(mtmp[:], cmask, W=[rmtmp])
        k.cp("pool", Mfox[:], mtmp[:], R=[rmtmp], W=[rMfox])
        for par in range(2):
            k.memset("pool", Vts[par][:, :, 64:65], 1.0, W=[rVs[par]])

        def prep(p):
            hp, kind = p // 2, p % 2
            par = p % 2
            Kt, Vt, Qt = Kts[par], Vts[par], Qts[par]
            st_ = {"hp": hp, "kind": kind, "par": par}
            k.dma(Kt[0:64, :], KVd[hp, kind, 0:64, :], R=[rKVd[hp][kind]], W=[rKs[par]])
            if kind == 0:
                k.memset("pool", Kt[64:67, :], 1.0, W=[rKs[par]])
                k.dma(Qt[0:64, :], Qd[hp, kind, 0:64, :], R=[rQd[hp][kind]], W=[rQs[par]])
                k.dma(Qt[64:67, :], C3d[hp], R=[rC3d], W=[rQs[par]])
                crow, rcrow = crow_ring.next()
                k.dma(crow[:], Crd[hp:hp + 1, :], R=[rCrd], W=[rcrow])
                crefB, rcrefB = crefB_ring.next()
                po, rpo = psO.next()
                k.mm(po[:, 0:NC2], onesf[0:1, 0:128], crow[0:1, :], R=[ronesf, rcrow], W=[rpo])
                k.cp("dve", crefB[:], po[:, 0:NC2], R=[rpo], W=[rcrefB])
                cdt, rcdt = cdt_ring.next()
                k.dma(cdt[:], Cd8[hp].rearrange("(t p) -> t p", p=128), R=[rCd8], W=[rcdt])
                negC, rnegC = negC_ring.next()
                po, rpo = psO.next()
                k.tr(po[:, 0:NT], cdt[:], idf[0:NT, 0:NT], R=[rcdt, ridf], W=[rpo])
                k.ts("dve", negC[:], po[:, 0:NT], -1.0, None, ALU.mult, R=[rpo], W=[rnegC])
                k.tt("dve", negC[:], negC[:], padks[:], ALU.add, R=[rnegC, rpadk], W=[rnegC])
                st_.update(crefB=crefB, rcrefB=rcrefB, negC=negC, rnegC=rnegC, Mk=Mfox, rMk=rMfox)
            else:
                k.dma(Kt[64:128, :], onehot, W=[rKs[par]])
                k.dma(Qt[:, :], Qd[hp, kind], R=[rQd[hp][kind]], W=[rQs[par]])
                mt2, rmt2 = mt2_ring.next()
                k.dma(mt2[:], mbias[hp], W=[rmt2])
                Mm, rMm = Mm_ring.next()
                k.tt("pool", Mm[:], mtmp[:], mt2[:], ALU.add, R=[rmtmp, rmt2], W=[rMm])
                kbf, rkbf = kbf_ring.next()
                k.ts("dve", kbf[:], padks[:], rb31s[:, hp:hp + 1], None, ALU.add, R=[rpadk, rrb31], W=[rkbf])
                st_.update(kbf=kbf, rkbf=rkbf, Mk=Mm, rMk=rMm)
            def vgroup(g):
                vts, rvts = vts_ring.next()
                k.dma(vts[64:128, :], KVd[hp, kind, 64:128, g * 512:(g + 1) * 512], R=[rKVd[hp][kind]], W=[rvts])
                pv, rpv = psT.next()
                for i in range(4):
                    k.tr(pv[:, i * 64:(i + 1) * 64], vts[64:128, i * 128:(i + 1) * 128], idb[64:128, 64:128],
                         R=[rvts, ridb], W=[rpv])
                k.cp("dve", Vt[:, 4 * g:4 * g + 4, 0:64], pv[:, 0:256].rearrange("p (a b) -> p a b", a=4),
                     R=[rpv], W=[rVs[par]])
            st_["vthunks"] = [(lambda g=g: vgroup(g)) for g in range(NQC)]
            return st_

        def attn(st_, thunks):
            hp, kind, par = st_["hp"], st_["kind"], st_["par"]
            npairs = sum(4 * qc + 4 for qc in OC)
            stride = max(1, npairs // (len(thunks) + 1)) if thunks else 0
            cnt = [0]
            Kt, Vt, Qt = Kts[par], Vts[par], Qts[par]
            rK1, rV1, rQ1 = rKs[par], rVs[par], rQs[par]
            KR = 67 if kind == 0 else 128
            Mk, rMk = st_["Mk"], st_["rMk"]
            LA = 2
            pending = [None]

            def finalize(oj, po, rpo):
                t0 = oj * 512
                ot, rot = ot_ring.next()
                k.cp("act", ot[0:65, :], po[0:65, :], R=[rpo], W=[rot])
                rl, rrl = rl_ring.next()
                k.recip(rl[64:65, :], ot[64:65, :], R=[rot], W=[rrl])
                pb, rpb = psP.next()
                k.mm(pb[0:64, :], onesf[64:65, 0:64], rl[64:65, :], R=[ronesf, rrl], W=[rpb])
                yt, ryt = yt_ring.next()
                k.tt("dve", yt[:], ot[0:64, :], pb[0:64, :], ALU.mult, R=[rot, rpb], W=[ryt])
                k.dma(mixd[hp, kind * 64:(kind + 1) * 64, t0:t0 + 512], yt[:], R=[ryt], W=[rMix], q="pool")

            for oj, qc in enumerate(OC):
                t0 = oj * 512
                nk = 4 * qc + 4
                if kind == 0:
                    bq, rbq = bq_ring.next()
                    k.ts("dve", bq[:, 0:nk], st_["negC"][:, 0:nk], st_["crefB"][:, qc:qc + 1], None, ALU.add,
                         R=[st_["rnegC"], st_["rcrefB"]], W=[rbq])
                po, rpo = psO.next()
                Ps = {}
                for it in range(nk + LA):
                    kt = it
                    if kt < nk:
                        j = kt - 4 * qc
                        near = (j >= 0) if kind == 0 else (j >= -1)
                        pS, rpS = psP.next()
                        k.mm(pS[:], Kt[0:KR, kt * 128:(kt + 1) * 128], Qt[0:KR, t0:t0 + 512], start=True,
                             stop=not near, R=[rK1, rQ1], W=[rpS])
                        if near:
                            s0_ = 384 - 128 * j
                            k.mm(pS[:], idb[:], Mk[:, s0_:s0_ + 512], start=False, stop=True, R=[ridb, rMk], W=[rpS])
                        P, rP = P_ring.next()
                        if kind == 0:
                            k.act(P[:], pS[:], AF.Exp, bias=bq[:, kt:kt + 1], R=[rpS, rbq], W=[rP])
                        elif near:
                            k.act(P[:], pS[:], AF.Exp, bias=kbn[:, kt:kt + 1], R=[rpS, rkbn], W=[rP])
                        else:
                            k.act(P[:], pS[:], AF.Exp, bias=st_["kbf"][:, kt:kt + 1], R=[rpS, st_["rkbf"]], W=[rP])
                        Ps[kt] = (P, rP)
                    if it == LA and pending[0] is not None:
                        finalize(*pending[0])
                        pending[0] = None
                    kv = it - LA
                    if kv >= 0:
                        P, rP = Ps.pop(kv)
                        k.mm(po[0:65, :], Vt[:, kv, 0:65], P[:], start=(kv == 0), stop=(kv == nk - 1),
                             R=[rV1, rP], W=[rpo])
                        cnt[0] += 1
                        if thunks and cnt[0] % stride == 0:
                            thunks.pop(0)()
                if pending[0] is not None:
                    finalize(*pending[0])
                pending[0] = (oj, po, rpo)
            if pending[0] is not None:
                finalize(*pending[0])
                pending[0] = None
            while thunks:
                thunks.pop(0)()

        nxt = prep(0)
        for th in nxt["vthunks"]:
            th()
        for p in range(16):
            cur = nxt
            thunks = []
            if p + 1 < 16:
                nxt = prep(p + 1)
                thunks = list(nxt["vthunks"])
            attn(cur, thunks)
        sch.flush()
        sA.close()

        sB = ExitStack()

        def mk_alloc(stack, prefix):
            def sb_(name, shape, dt=F32):
                return stack.enter_context(nc.sbuf_tensor(prefix + name, shape, dt))
            return sb_
        sb = mk_alloc(sB, "B_")

        def pst(name, shape, dt=F32):
            return sB.enter_context(nc.psum_tensor("B_" + name, shape, dt))

        psA = Ring([pst("psA%d" % i, [128, 512]) for i in range(8)], x=True)
        idf = sb("idf", [128, 128]); ridf = Res()
        onesf = sb("onesf", [128, 128]); ronesf = Res()
        onesb = sb("onesb", [128, 128], BF16); ronesb = Res()
        h2Tb = sb("h2Tb", [128, 8, TPC], BF16); rh2b = [Res() for _ in range(NTL)]
        Gt = sb("Gts", [128, NTL, NE]); rGt = Res()
        GF = sb("GF", [128, D]); rGF = Res()
        bguT = sb("bguT", [128, 16 * NE]); rbguT = Res()
        rX1 = Res()
        rOut = Res()
        k.dma(idf[:], identf, W=[ridf])
        k.memset("pool", onesf[:], 1.0, W=[ronesf])
        k.memset("pool", onesb[:], 1.0, W=[ronesb])

        with ExitStack() as s1:
            sb1 = mk_alloc(s1, "B1_")
            vin = sb1("vin", [64, 128]); rvin = Res()
            vT = sb1("vT", [128, 64]); rvT = Res()
            cond2 = sb1("cond2", [128, 8, 2]); rcond = Res()
            modT = sb1("modT", [128, 64]); rmod = Res()
            modf = sb1("modf", [128, 32]); rmodf = Res()
            v8 = sb1("v8", [128, 4, 8]); rv8 = Res()
            dg = sb1("dg", [128, 8, 128]); rdg = Res()
            GG = sb1("GG", [128, D]); rGG = Res()
            G2 = sb1("G2", [128, D]); rG2 = Res()
            SH2 = sb1("SH2", [128, D]); rSH2 = Res()
            bgus = sb1("bgus", [NE, 2 * D]); rbgus = Res()
            wr = sb1("wrs", [128, 8, NE]); rwr = Res()
            brs = sb1("brs", [1, NE]); rbrs = Res()
            mixT = sb1("mixTs", [128, 8, TPC], BF16); rmix = Res()
            wob = sb1("wob", [128, 8, D], BF16); rwob = Res()
            L = sb1("L", [128, NTL, NE]); rL = Res()
            Self_ = sb1("Self", [128, NTL, NE]); rSelf = Res()
            junkb = sb1("junkb", [128, D], BF16); rjunk = Res()
            ssA = sb1("ssA", [128, 4]); rssA = Res()
            st_ring = Ring([sb1("stg%d" % i, [128, D]) for i in range(2)])
            wa_ring = Ring([sb1("wa%d" % i, [128, 8, 512]) for i in range(2)])
            xt_ring = Ring([sb1("xt%d" % i, [128, D]) for i in range(3)])
            x1_ring = Ring([sb1("x1%d" % i, [128, D]) for i in range(2)])
            h2f_ring = Ring([sb1("h2f%d" % i, [128, D]) for i in range(2)])
            h2T_ring = Ring([sb1("h2T%d" % i, [128, 8, 128]) for i in range(2)])
            sm_ring = Ring([sb1("sm%d" % i, [128, 8 + 4 * NE + 8]) for i in range(2)])

            k.dma(vin[:], vecsB, W=[rvin])
            k.dma(bgus[:], bgu_d, W=[rbgus])
            k.dma(wr[:], wr_d.rearrange("(k p) n -> p k n", p=128), W=[rwr])
            k.dma(brs[:], br_d, W=[rbrs])
            for r in range(8):
                k.dma(mixT[:, r, :], mixd[r], R=[rMix], W=[rmix])
            for r in range(8):
                sg, rsg = st_ring.next()
                k.dma(sg[:], wout[r], W=[rsg])
                k.cp("pool", wob[:, r, :], sg[:], R=[rsg], W=[rwob])
            po, rpo = psA.next()
            k.tr(po[:, 0:64], vin[:], idf[0:64, 0:64], R=[rvin, ridf], W=[rpo])
            k.cp("dve", vT[:], po[:, 0:64], R=[rpo], W=[rvT])
            k.memset("pool", cond2[:], 0.0, W=[rcond])
            k.act(cond2[:, :, 0], vT[:, 0:8], AF.Silu, R=[rvT], W=[rcond])
            po, rpo = psA.next()
            for blk in range(8):
                wa, rwa = wa_ring.next()
                k.dma(wa[:], wadaB[:, blk * 512:(blk + 1) * 512].rearrange("(k p) n -> p k n", p=128), W=[rwa])
                for jj in range(4):
                    j = blk * 4 + jj
                    for kk in range(8):
                        k.mm(po[:, 2 * j:2 * j + 2], wa[:, kk, jj * 128:(jj + 1) * 128], cond2[:, kk, :],
                             start=(kk == 0), stop=(kk == 7), R=[rwa, rcond], W=[rpo])
            k.cp("dve", modT[:], po[:, 0:64], R=[rpo], W=[rmod])
            modv = modT[:].rearrange("p (j t) -> p j t", t=2)
            k.tt("dve", modf[:], modv[:, :, 0], vT[:, 8:40], ALU.add, R=[rmod, rvT], W=[rmodf])
            k.tt("dve", v8[:, 0, :], modf[:, 0:8], vT[:, 40:48], ALU.mult, R=[rmodf, rvT], W=[rv8])
            k.ts("dve", v8[:, 1, :], modf[:, 16:24], 1.0, None, ALU.add, R=[rmodf], W=[rv8])
            k.tt("dve", v8[:, 1, :], v8[:, 1, :], vT[:, 48:56], ALU.mult, R=[rv8, rvT], W=[rv8])
            k.cp("dve", v8[:, 2, :], modf[:, 8:16], R=[rmodf], W=[rv8])
            k.tt("dve", v8[:, 3, :], modf[:, 24:32], vT[:, 56:64], ALU.mult, R=[rmodf, rvT], W=[rv8])
            for vi, (dst, rdst) in enumerate(((GG, rGG), (G2, rG2), (SH2, rSH2), (GF, rGF))):
                for kk in range(8):
                    k.ts("dve", dg[:, kk, :], idf[:], v8[:, vi, kk:kk + 1], None, ALU.mult, R=[ridf, rv8], W=[rdg])
                for half in range(2):
                    po, rpo = psA.next()
                    k.mm(po[:], onesf[:], dg[:, 4 * half:4 * half + 4, :], R=[ronesf, rdg], W=[rpo])
                    k.cp("act", dst[:, half * 512:(half + 1) * 512], po[:], R=[rpo], W=[rdst])
            po, rpo = psA.next()
            for fc in range(16):
                k.tr(po[:, fc * NE:(fc + 1) * NE], bgus[:, fc * 128:(fc + 1) * 128], idf[0:NE, 0:NE],
                     R=[rbgus, ridf], W=[rpo])
            k.cp("dve", bguT[:], po[:], R=[rpo], W=[rbguT])

            for i in range(NTL):
                tc = slice(i * 128, (i + 1) * 128)
                xt, rxt = xt_ring.next()
                k.dma(xt[:], x[xrow(i):xrow(i) + 128, :], W=[rxt])
                pp = []
                for half in range(2):
                    po, rpo = psA.next()
                    for r in range(8):
                        k.mm(po[:], mixT[:, r, tc], wob[:, r, half * 512:(half + 1) * 512],
                             start=(r == 0), stop=(r == 7), R=[rmix, rwob], W=[rpo])
                    k.act(junkb[:, 0:512], po[:], AF.Square, accum=ssA[:, half:half + 1], R=[rpo], W=[rjunk, rssA])
                    pp.append((po, rpo))
                sm, rsm = sm_ring.next()
                k.tt("dve", sm[:, 0:1], ssA[:, 0:1], ssA[:, 1:2], ALU.add, R=[rssA], W=[rsm])
                k.act(sm[:, 1:2], sm[:, 0:1], AF.Sqrt, bias=1e-6, scale=1.0 / D, R=[rsm], W=[rsm])
                k.recip(sm[:, 2:3], sm[:, 1:2], R=[rsm], W=[rsm])
                x1, rx1 = x1_ring.next()
                for half in range(2):
                    po, rpo = pp[half]
                    hs = slice(half * 512, (half + 1) * 512)
                    k.stt(x1[:, hs], po[:], sm[:, 2:3], GG[:, hs], ALU.mult, ALU.mult, R=[rpo, rsm, rGG], W=[rx1])
                k.tt("dve", x1[:], x1[:], xt[:], ALU.add, R=[rx1, rxt], W=[rx1])
                k.dma(X1d[tc, :], x1[:], R=[rx1], W=[rX1], q="pool")
                k.act(junkb[:], x1[:], AF.Square, accum=ssA[:, 2:3], R=[rx1], W=[rjunk, rssA])
                k.act(sm[:, 3:4], ssA[:, 2:3], AF.Sqrt, bias=1e-6, scale=1.0 / D, R=[rssA], W=[rsm])
                k.recip(sm[:, 4:5], sm[:, 3:4], R=[rsm], W=[rsm])
                h2f, rh2f = h2f_ring.next()
                k.stt(h2f[:], x1[:], sm[:, 4:5], G2[:], ALU.mult, ALU.mult, R=[rx1, rsm, rG2], W=[rh2f])
                k.tt("dve", h2f[:], h2f[:], SH2[:], ALU.add, R=[rh2f, rSH2], W=[rh2f])
                h2T, rh2T = h2T_ring.next()
                for half in range(2):
                    po, rpo = psA.next()
                    for kk in range(4):
                        c0 = (half * 4 + kk) * 128
                        k.tr(po[:, kk * 128:(kk + 1) * 128], h2f[:, c0:c0 + 128], idf[:], R=[rh2f, ridf], W=[rpo])
                    k.cp("act", h2T[:, half * 4:half * 4 + 4, :], po[:].rearrange("p (k t) -> p k t", k=4),
                         R=[rpo], W=[rh2T])
                    k.cp("dve", h2Tb[:, half * 4:half * 4 + 4, tc], po[:].rearrange("p (k t) -> p k t", k=4),
                         R=[rpo], W=[rh2b[i]])
                po, rpo = psA.next()
                for kk in range(8):
                    k.mm(po[:, 0:NE], h2T[:, kk, :], wr[:, kk, :], start=(kk == 0), stop=False, R=[rh2T, rwr], W=[rpo])
                k.mm(po[:, 0:NE], onesf[0:1, :], brs[0:1, :], start=False, stop=True, R=[ronesf, rbrs], W=[rpo])
                k.cp("dve", L[:, i, :], po[:, 0:NE], R=[rpo], W=[rL])
                mx = sm[:, 8:16]
                k.s.op("dve", lambda e, mx=mx, Li=L[:, i, :]: e.max(mx, Li), [rL], [rsm])
                k.ts("dve", Self_[:, i, :], L[:, i, :], mx[:, 3:4], None, ALU.is_ge, R=[rL, rsm], W=[rSelf])
                k.ts("dve", sm[:, 5:6], mx[:, 0:1], -1.0, None, ALU.mult, R=[rsm], W=[rsm])
                ex = sm[:, 16:16 + NE]
                ew = sm[:, 16 + NE:16 + 2 * NE]
                k.act(ex, L[:, i, :], AF.Exp, bias=sm[:, 5:6], R=[rL, rsm], W=[rsm])
                k.tt("dve", ew, ex, Self_[:, i, :], ALU.mult, R=[rsm, rSelf], W=[rsm])
                k.s.op("dve", lambda e, z=sm[:, 6:7], ew=ew: e.tensor_reduce(z, ew, AX.X, ALU.add), [rsm], [rsm])
                k.recip(sm[:, 7:8], sm[:, 6:7], R=[rsm], W=[rsm])
                k.ts("dve", Gt[:, i, :], ew, sm[:, 7:8], None, ALU.mult, R=[rsm], W=[rGt])
            sch.flush()

        with ExitStack() as s2:
            sb2 = mk_alloc(s2, "B2_")
            CH = min(512, TPC)
            NCH = TPC // CH
            yacc = sb2("yacc", [128, NTL, D]); ryacc = [Res() for _ in range(NTL)]
            actT = sb2("actT", [128, 8, TPC], BF16); ract = [Res() for _ in range(NCH)]
            bdall = sb2("bdall", [NE, D]); rbdall = Res()
            gtT_ring = Ring([sb2("gtT%d" % i, [NE, 128]) for i in range(2)])
            w_ring = Ring([sb2("wu%d" % i, [128, 8, 512], BF16) for i in range(4)])
            sg_ring = Ring([sb2("sg%d" % i, [128, 512]) for i in range(4)])
            xg_t = sb2("xg", [128, CH]); rxg = Res()
            sig_t = sb2("sig", [128, CH]); rsig = Res()
            xl_t = sb2("xl", [128, CH]); rxl = Res()
            xt_ring = Ring([sb2("xf%d" % i, [128, D]) for i in range(2)])
            sm2 = Ring([sb2("sq%d" % i, [128, 4]) for i in range(2)])
            junk2 = sb2("junk2", [128, D], BF16); rjunk2 = Res()

            k.memset("pool", yacc[:], 0.0, W=ryacc)
            k.dma(bdall[:], bd_d, W=[rbdall])

            def load_unit_q(e, q):
                w, rw = w_ring.next()
                for kk in range(8):
                    sg, rsg = sg_ring.next()
                    src = wgu_d[e, kk * 128:(kk + 1) * 128, :].rearrange("p (two c) -> p two c", two=2)
                    k.dma(sg[:].rearrange("p (two c) -> p two c", two=2), src[:, :, q * 256:(q + 1) * 256], W=[rsg])
                    k.cp("pool", w[:, kk, :], sg[:], R=[rsg], W=[rw])
                return w, rw

            def load_unit_d(e, nh):
                w, rw = w_ring.next()
                for fk in range(8):
                    sg, rsg = sg_ring.next()
                    k.dma(sg[:], wd_d[e, fk * 128:(fk + 1) * 128, nh * 512:(nh + 1) * 512], W=[rsg])
                    k.cp("pool", w[:, fk, :], sg[:], R=[rsg], W=[rw])
                return w, rw

            for e in range(NE):
                for q in range(4):
                    w, rw = load_unit_q(e, q)
                    for ch in range(NCH):
                        cs = slice(ch * CH, (ch + 1) * CH)
                        for f2 in range(2):
                            fcl = q * 2 + f2
                            pgt, rpgt = psA.next()
                            for kk in range(8):
                                k.mm(pgt[:, 0:CH], w[:, kk, f2 * 128:(f2 + 1) * 128], h2Tb[:, kk, cs],
                                     start=(kk == 0), stop=(kk == 7), R=[rw] + rh2b, W=[rpgt])
                            plt, rplt = psA.next()
                            for kk in range(8):
                                k.mm(plt[:, 0:CH], w[:, kk, 256 + f2 * 128:256 + (f2 + 1) * 128], h2Tb[:, kk, cs],
                                     start=(kk == 0), stop=(kk == 7), R=[rw] + rh2b, W=[rplt])
                            bg = bguT[:, fcl * NE + e:fcl * NE + e + 1]
                            bl = bguT[:, (8 + fcl) * NE + e:(8 + fcl) * NE + e + 1]
                            k.ts("dve", xg_t[:], pgt[:, 0:CH], bg, 7.0, ALU.add, ALU.min, R=[rpgt, rbguT], W=[rxg])
                            k.act(sig_t[:], xg_t[:], AF.Sigmoid, scale=1.702, R=[rxg], W=[rsig])
                            k.ts("dve", xl_t[:], plt[:, 0:CH], bl, 7.0, ALU.add, ALU.min, R=[rplt, rbguT], W=[rxl])
                            k.ts("dve", xl_t[:], xl_t[:], -7.0, 1.0, ALU.max, ALU.add, R=[rxl], W=[rxl])
                            k.tt("dve", xg_t[:], xg_t[:], sig_t[:], ALU.mult, R=[rxg, rsig], W=[rxg])
                            k.tt("dve", actT[:, fcl, cs], xg_t[:], xl_t[:], ALU.mult, R=[rxg, rxl], W=[ract[ch]])
                for nh in range(2):
                    w, rw = load_unit_d(e, nh)
                    for i in range(NTL):
                        tc = slice(i * 128, (i + 1) * 128)
                        py, rpy = psA.next()
                        for fk in range(8):
                            k.mm(py[:], actT[:, fk, tc], w[:, fk, :], start=(fk == 0), stop=(fk == 7),
                                 R=[ract[(i * 128) // CH], rw], W=[rpy])
                        ys = yacc[:, i, nh * 512:(nh + 1) * 512]
                        k.stt(ys, py[:], Gt[:, i, e:e + 1], ys, ALU.mult, ALU.add, R=[rpy, rGt, ryacc[i]], W=[ryacc[i]])
            for i in range(NTL):
                tc = slice(i * 128, (i + 1) * 128)
                xt, rxt = xt_ring.next()
                k.dma(xt[:], X1d[tc, :], R=[rX1], W=[rxt])
                pg_, rpg_ = psA.next()
                k.tr(pg_[0:NE, 0:128], Gt[:, i, :], idf[:], R=[rGt, ridf], W=[rpg_])
                gtT, rgtT = gtT_ring.next()
                k.cp("dve", gtT[:], pg_[0:NE, 0:128], R=[rpg_], W=[rgtT])
                for nh in range(2):
                    pb_, rpb_ = psA.next()
                    k.mm(pb_[:], gtT[:], bdall[:, nh * 512:(nh + 1) * 512], R=[rgtT, rbdall], W=[rpb_])
                    ys = yacc[:, i, nh * 512:(nh + 1) * 512]
                    k.tt("dve", ys, ys, pb_[:], ALU.add, R=[ryacc[i], rpb_], W=[ryacc[i]])
                sq, rsq = sm2.next()
                k.act(junk2[:], yacc[:, i, :], AF.Square, accum=sq[:, 0:1], R=[ryacc[i]], W=[rjunk2, rsq])
                k.act(sq[:, 1:2], sq[:, 0:1], AF.Sqrt, bias=1e-6, scale=1.0 / D, R=[rsq], W=[rsq])
                k.recip(sq[:, 2:3], sq[:, 1:2], R=[rsq], W=[rsq])
                k.stt(yacc[:, i, :], yacc[:, i, :], sq[:, 2:3], GF[:], ALU.mult, ALU.mult, R=[ryacc[i], rsq, rGF], W=[ryacc[i]])
                k.tt("dve", yacc[:, i, :], yacc[:, i, :], xt[:], ALU.add, R=[ryacc[i], rxt], W=[ryacc[i]])
                k.dma(out[tc, :], yacc[:, i, :], R=[ryacc[i]], W=[rOut])
            sch.flush(final=[rOut])
        sB.close()
    return nc


def t5_bucket_np(dist):
    dist = np.maximum(dist, 0)
    max_exact = 16
    d = np.maximum(dist, 1).astype(np.float32)
    large = max_exact + (np.log(d / np.float32(max_exact)) / np.float32(np.log(128 / 16)) * np.float32(16)).astype(np.int32)
    large = np.minimum(large, 31)
    return np.where(dist < max_exact, dist, large)


def fused_inputs(S, inp):
    x = inp["x"].reshape(S, D)
    NT = S // 128
    W = inp["w_in"][0]
    win = np.stack([np.ascontiguousarray(W[:, np.concatenate([
        np.arange(h * 64, h * 64 + 64), 512 + np.arange(h * 64, h * 64 + 64),
        1024 + np.arange(h * 64, h * 64 + 64), 1536 + np.arange(h * 64, h * 64 + 64),
        2048 + np.arange(h * 64, h * 64 + 64), 2560 + np.arange(h * 64, h * 64 + 64),
        np.array([3072 + h])])]) for h in range(8)], axis=0)
    w_ada = inp["w_ada"][0]
    ba = inp["b_ada"][0]
    c = inp["c"]
    vecsA = np.concatenate([c.reshape(8, 128), inp["g_pre_mix"][0].reshape(8, 128), ba[0:1024].reshape(8, 128),
                            ba[1024:2048].reshape(8, 128)], axis=0).astype(np.float32)
    vecsB = np.concatenate([c.reshape(8, 128), ba[2048:6144].reshape(32, 128), inp["g_post_mix"][0].reshape(8, 128),
                            inp["g_pre_ffn"][0].reshape(8, 128), inp["g_post_ffn"][0].reshape(8, 128)],
                           axis=0).astype(np.float32)
    p = np.arange(128)[:, None]
    xx = np.arange(1024)[None, :]
    delta = xx - 384 - p
    cmask = np.where(delta >= 0, 0.0, NEG).astype(np.float32)
    bucket = t5_bucket_np(delta)
    rel_bias = inp["rel_bias"]
    mbias = np.stack([rel_bias[bucket, h] for h in range(8)], axis=0).astype(np.float32)
    rb31 = np.tile(rel_bias[31, :][None, :], (128, 1)).astype(np.float32)
    oh = np.zeros((64, S), dtype=ml_dtypes.bfloat16)
    for j in range(min(S // 256, 64)):
        oh[j, j * 256:(j + 1) * 256] = 1.0
    Wo = inp["w_out"][0]
    wout = np.stack([np.concatenate([Wo[r * 64:(r + 1) * 64], Wo[512 + r * 64:512 + (r + 1) * 64]], axis=0)
                     for r in range(8)], axis=0)
    shared = {"win": win, "wadaA": np.ascontiguousarray(w_ada[:, 0:2048]), "vecsA": vecsA,
              "bfg": inp["b_forget"][0].reshape(1, 8).astype(np.float32), "mbias": mbias, "cmask": cmask,
              "rb31": rb31, "onehot": oh, "identf": np.eye(128, dtype=np.float32),
              "wout": wout, "wadaB": np.ascontiguousarray(w_ada[:, 2048:6144]), "vecsB": vecsB,
              "wr": inp["w_router"][0], "br": inp["b_router"][0].reshape(1, NE),
              "wgu": inp["w_gate_up"][0], "bgu": inp["b_gate_up"][0], "wd": inp["w_down"][0], "bd": inp["b_down"][0]}
    maps = []
    for cix in range(NCORES):
        pad = 512 * (7 - cix)
        xw = np.zeros((S, D), dtype=np.float32)
        xw[pad:] = x[0:S - pad]
        padk = np.zeros((128, NT), dtype=np.float32)
        padk[:, 0:pad // 128] = NEG
        padG = np.zeros((128, 64), dtype=np.float32)
        padG[:, 0:pad // 256] = -1e30
        m = dict(shared)
        m.update({"x": xw, "padk": padk, "padG": padG})
        maps.append(m)
    return maps


_CACHE = {}


def run_fused(inp, dbg=False):
    S = inp["x"].shape[1]
    key = ("f", S, dbg)
    if key not in _CACHE:
        _CACHE[key] = build_fused(S, dbg=dbg)
    nc = _CACHE[key]
    maps = fused_inputs(S, inp)
    res = run_bass_kernel_spmd(nc, maps, core_ids=list(range(NCORES)))
    NOC = (S // 512) // 8
    out = np.zeros((S, D), dtype=np.float32)
    for cix in range(NCORES):
        o = np.asarray(res.results[cix]["out"])
        for j in range(NOC):
            rc = cix + 8 * j
            out[rc * 512:(rc + 1) * 512] = o[j * 512:(j + 1) * 512]
    return out, res


def kernel(**inputs):
    inp = {k_: np.asarray(v) for k_, v in inputs.items()}
    S = inp["x"].shape[1]
    out, _ = run_fused(inp)
    return out.reshape(1, S, D).astype(np.float32)
```

```python
import numpy as np
import ml_dtypes
from contextlib import ExitStack
import concourse.bass as bass
import concourse.mybir as mybir
from concourse.bass_utils import run_bass_kernel_spmd

F32 = mybir.dt.float32
BF16 = mybir.dt.bfloat16
AF = mybir.ActivationFunctionType
ALU = mybir.AluOpType
AX = mybir.AxisListType
NEG = -30000.0
D = 1024
NCORES = 8


class Res:
    __slots__ = ("name", "w", "r", "x")

    def __init__(self, name="", x=False):
        self.name = name
        self.w = None
        self.r = {}
        self.x = x


class Sched:
    EPOCH = 12000
    NDMA = 8

    def __init__(self, nc, stack):
        self.nc = nc
        self.stack = stack
        self.names = ["pe", "act", "dve", "pool", "sp"]
        self.cnt = {k: 0 for k in self.names}
        self.sems = {}
        self.ops = {k: [] for k in self.names}
        self.waited = {k: {} for k in self.names}
        self.dma_i = {k: 0 for k in self.names}

    def sem(self, key):
        if key not in self.sems:
            self.sems[key] = self.stack.enter_context(
                self.nc.semaphore("s_" + "_".join(str(k) for k in key)))
        return self.sems[key]

    def _need(self, e, ev, waits):
        if ev is None:
            return
        key, val = ev
        if key[0] == "c" and key[1] == e and e == "pe":
            return
        if self.waited[e].get(key, 0) >= val:
            return
        self.waited[e][key] = val
        waits.append((key, val))

    def op(self, e, fn, reads=(), writes=(), dma=False):
        writes = list(writes) + [r for r in reads if r.x]
        reads = [r for r in reads if not r.x]
        waits = []
        for r in reads:
            self._need(e, r.w, waits)
        for r in writes:
            self._need(e, r.w, waits)
            for k, v in r.r.items():
                self._need(e, (k, v), waits)
        if dma:
            i = self.dma_i[e]
            self.dma_i[e] += 1
            key = ("d", e, i % self.NDMA)
            val = 16 * (i // self.NDMA + 1)
            if i >= self.NDMA:
                self._need(e, (key, val - 16), waits)
            inc = 16
        else:
            k = self.cnt[e]
            self.cnt[e] += 1
            key = ("c", e, k // self.EPOCH)
            val = k % self.EPOCH + 1
            inc = 1
        ev = (key, val)
        self.sem(key)
        for (k2, v2) in waits:
            self.sem(k2)
        self.ops[e].append((waits, fn, key, inc))
        for r in reads:
            r.r[key] = max(r.r.get(key, 0), val)
        for r in writes:
            r.w = ev
            r.r = {}
        return ev

    def flush(self, final=()):
        nc = self.nc
        ops = self.ops
        sems = self.sems
        fin = [r.w for r in final if r.w is not None]
        with nc.Block() as block:
            def mk(ename):
                def body(eng):
                    for (waits, fn, key, inc) in ops[ename]:
                        for (k2, v2) in waits:
                            eng.wait_ge(sems[k2], v2)
                        fn(eng).then_inc(sems[key], inc)
                    if ename == "sp":
                        for (k2, v2) in fin:
                            eng.wait_ge(sems[k2], v2)
                        for q in self.names:
                            n = self.dma_i[q]
                            for j in range(min(n, self.NDMA)):
                                cntj = (n - 1 - j) // self.NDMA + 1
                                eng.wait_ge(sems[("d", q, j)], 16 * cntj)
                return body
            block.tensor(mk("pe"))
            block.scalar(mk("act"))
            block.vector(mk("dve"))
            block.gpsimd(mk("pool"))
            block.sync(mk("sp"))
        self.ops = {k: [] for k in self.names}


class Ring:
    def __init__(self, items, x=False):
        self.items = items
        self.res = [Res(x=x) for _ in items]
        self.i = 0

    def next(self):
        k = self.i % len(self.items)
        self.i += 1
        return self.items[k], self.res[k]


class K:
    def __init__(self, sch):
        self.s = sch

    def mm(self, out, lhsT, rhs, start=True, stop=True, R=(), W=()):
        self.s.op("pe", lambda e: e.matmul(out, lhsT=lhsT, rhs=rhs, start=start, stop=stop), R, W)

    def tr(self, out, in_, ident, R=(), W=()):
        self.s.op("pe", lambda e: e.transpose(out, in_, ident), R, W)

    def act(self, out, in_, func, bias=None, scale=None, accum=None, R=(), W=()):
        kw = {}
        if bias is not None:
            kw["bias"] = bias
        if scale is not None:
            kw["scale"] = scale
        if accum is not None:
            kw["accum_out"] = accum
        self.s.op("act", lambda e: e.activation(out=out, in_=in_, func=func, **kw), R, W)

    def ts(self, eng, out, in0, s1, s2, op0, op1=None, R=(), W=()):
        if op1 is None:
            self.s.op(eng, lambda e: e.tensor_scalar(out, in0, s1, None, op0), R, W)
        else:
            self.s.op(eng, lambda e: e.tensor_scalar(out, in0, s1, s2, op0, op1), R, W)

    def tt(self, eng, out, in0, in1, op, R=(), W=()):
        self.s.op(eng, lambda e: e.tensor_tensor(out, in0, in1, op), R, W)

    def stt(self, out, in0, scalar, in1, op0, op1, R=(), W=()):
        self.s.op("dve", lambda e: e.scalar_tensor_tensor(out, in0, scalar, in1, op0, op1), R, W)

    def cp(self, eng, out, in_, R=(), W=()):
        if eng == "act":
            self.s.op("act", lambda e: e.activation(out=out, in_=in_, func=AF.Copy), R, W)
        else:
            self.s.op(eng, lambda e: e.tensor_copy(out, in_), R, W)

    def memset(self, eng, out, val, R=(), W=()):
        self.s.op(eng, lambda e: e.memset(out, val), R, W)

    def recip(self, out, in_, R=(), W=()):
        self.s.op("dve", lambda e: e.reciprocal(out, in_), R, W)

    def dma(self, out, in_, R=(), W=(), q="sp", **kw):
        self.s.op(q, lambda e: e.dma_start(out=out, in_=in_, **kw), R, W, dma=True)


NE = 32


def build_fused(S, dbg=False):
    NT = S // 128
    NQC = S // 512
    NB = S // 256
    NBP = max(NB, 8)
    NOC = NQC // 8
    OC = [7 + 8 * j for j in range(NOC)]
    TPC = NOC * 512
    NTL = TPC // 128
    nc = bass.Bass("TRN2", target_bir_lowering=False)

    def din(name, shape, dt=F32):
        return nc.dram_tensor(name, shape, dt, kind="ExternalInput").ap()
    x = din("x", [S, D])
    win = din("win", [8, D, 385])
    wada = din("wadaA", [D, 2048])
    vecs = din("vecsA", [32, 128])
    bfg = din("bfg", [1, 8])
    mbias = din("mbias", [8, 128, 1024])
    cmask = din("cmask", [128, 1024])
    rb31 = din("rb31", [128, 8])
    onehot = din("onehot", [64, S], BF16)
    identf = din("identf", [128, 128])
    padk = din("padk", [128, NT])
    padG = din("padG", [128, 64])
    wout = din("wout", [8, 128, D])
    wadaB = din("wadaB", [D, 4096])
    vecsB = din("vecsB", [64, 128])
    wr_d = din("wr", [D, NE])
    br_d = din("br", [1, NE])
    wgu_d = din("wgu", [NE, D, 2 * D])
    bgu_d = din("bgu", [NE, 2 * D])
    wd_d = din("wd", [NE, D, D])
    bd_d = din("bd", [NE, D])
    out = nc.dram_tensor("out", [TPC, D], F32, kind="ExternalOutput").ap()
    mixd = nc.dram_tensor("mixd", [8, 128, TPC], BF16, kind=("ExternalOutput" if dbg else "Internal")).ap()
    X1d = nc.dram_tensor("X1d", [TPC, D], F32, kind="Internal").ap()

    def xrow(i):
        return 128 * (4 * OC[i // 4] + i % 4)

    with ExitStack() as st:
        sch = Sched(nc, st)
        k = K(sch)
        rMix = Res()
        KM = 9

        KVd = nc.dram_tensor("KVd", [8, 2, 128, S], BF16, kind="Internal").ap()
        Qd = nc.dram_tensor("Qd", [8, 2, 128, TPC], BF16, kind="Internal").ap()
        C3d = nc.dram_tensor("C3d", [8, 3, TPC], BF16, kind="Internal").ap()
        Cd8 = nc.dram_tensor("Cd8", [8, S], F32, kind="Internal").ap()
        NC2 = max(NQC, 2)
        Crd = nc.dram_tensor("Crd", [8, NC2], F32, kind="Internal").ap()
        rKVd = [[Res() for _ in range(2)] for _ in range(8)]
        rQd = [[Res() for _ in range(2)] for _ in range(8)]
        rC3d = Res(); rCd8 = Res(); rCrd = Res()

        sW = ExitStack()

        def sb(name, shape, dt=F32):
            return sW.enter_context(nc.sbuf_tensor("W_" + name, shape, dt))

        def pst(name, shape, dt=F32):
            return sW.enter_context(nc.psum_tensor("W_" + name, shape, dt))

        psT = Ring([pst("psT%d" % i, [128, 1024], BF16) for i in range(2)], x=True)
        psP = Ring([pst("psP%d" % i, [128, 512]) for i in range(5)], x=True)
        psO = Ring([pst("psO%d" % i, [128, 512]) for i in range(1)], x=True)

        Wall = sb("Wall", [128, 8, 8 * 385], BF16); rWall = Res()
        ball = sb("ball", [128, 64]); rball = Res()
        Wf32 = sb("Wf32", [128, 8, 8]); rWf32 = Res()
        Wfb = sb("Wfb", [128, 8, 8], BF16); rWfb = Res()
        idf = sb("idf", [128, 128]); ridf = Res()
        idb = sb("idb", [128, 128], BF16); ridb = Res()
        onesf = sb("onesf", [128, 512]); ronesf = Res()
        vT = sb("vT", [128, 32]); rvT = Res()
        vin = sb("vin", [32, 128]); rvin = Res()
        cond2 = sb("cond2", [128, 8, 2]); rcond = Res()
        sh2 = sb("sh2", [128, 8, 2]); rsh2 = Res()
        modT = sb("modT", [128, 32]); rmod = Res()
        gm = sb("gm", [128, 8]); rgm = Res()
        negfb8 = sb("negfb8", [8, 2]); rnegfb = Res()
        bf8 = sb("bf8", [8, 1]); rbf8 = Res()
        padGs = sb("padGs", [128, 64]); rpadG = Res()
        ss = sb("ss", [128, 4]); rss = Res()
        rstd = sb("rstd", [128, 4]); rrstd = Res()
        crefrow8 = sb("crefrow8", [8, NC2]); rcrefrow = Res()
        kmall = sb("kmall", [64, 8 * NBP]); rkmean = Res()
        km2 = sb("km2", [64, 2]); rkm2 = Res()
        W32_ring = Ring([sb("W32_%d" % i, [128, 8, 385]) for i in range(2)])
        wada_ring = Ring([sb("wa%d" % i, [128, 8, 512]) for i in range(1)])
        xt_ring = Ring([sb("xt%d" % i, [128, D]) for i in range(9)])
        junk = sb("junk", [128, D], BF16); rjunk = Res()
        xn_ring = Ring([sb("xn%d" % i, [128, D], BF16) for i in range(4)])
        xT_ring = Ring([sb("xT%d" % i, [128, 8, 512], BF16) for i in range(3)])
        kst_ring = Ring([sb("kst%d" % i, [128, 512], BF16) for i in range(6)])
        qst_ring = Ring([sb("qst%d" % i, [128, 512], BF16) for i in range(3)])
        q32_ring = Ring([sb("q32%d" % i, [64, 512]) for i in range(2)])
        fr_ring = Ring([sb("fr%d" % i, [8, 4, 512]) for i in range(1)])
        c_ring = Ring([sb("cr%d" % i, [8, 512]) for i in range(2)])
        c3_ring = Ring([sb("c3%d" % i, [8, 3, 512], BF16) for i in range(2)])
        G_ring = Ring([sb("G%d" % i, [128, 64]) for i in range(2)])
        mx_ring = Ring([sb("mx%d" % i, [128, 8]) for i in range(2)])
        MB_ring = Ring([sb("MB%d" % i, [128, 128], BF16) for i in range(2)])
        MBf_ring = Ring([sb("MBf%d" % i, [128, 128]) for i in range(2)])

        k.dma(idf[:], identf, W=[ridf])
        k.cp("dve", idb[:], idf[:], R=[ridf], W=[ridb])
        k.memset("pool", onesf[:], 1.0, W=[ronesf])
        k.dma(vin[:], vecs, W=[rvin])
        k.dma(bf8[:], bfg.rearrange("o h -> h o"), W=[rbf8])
        k.dma(padGs[:], padG, W=[rpadG])
        k.memset("dve", kmall[:], 0.0, W=[rkmean])
        k.memset("dve", crefrow8[:], 0.0, W=[rcrefrow])
        po, rpo = psO.next()
        k.tr(po[:, 0:32], vin[:], idf[0:32, 0:32], R=[rvin, ridf], W=[rpo])
        k.cp("dve", vT[:], po[:, 0:32], R=[rpo], W=[rvT])
        k.memset("pool", cond2[:], 0.0, W=[rcond])
        k.memset("pool", sh2[:], 0.0, W=[rsh2])
        k.act(cond2[:, :, 0], vT[:, 0:8], AF.Silu, R=[rvT], W=[rcond])
        po, rpo = psO.next()
        for blk in range(4):
            wa, rwa = wada_ring.next()
            k.dma(wa[:], wada[:, blk * 512:(blk + 1) * 512].rearrange("(k p) n -> p k n", p=128), W=[rwa])
            for jj in range(4):
                j = blk * 4 + jj
                for kk in range(8):
                    k.mm(po[:, 2 * j:2 * j + 2], wa[:, kk, jj * 128:(jj + 1) * 128], cond2[:, kk, :],
                         start=(kk == 0), stop=(kk == 7), R=[rwa, rcond], W=[rpo])
        k.cp("dve", modT[:], po[:, 0:32], R=[rpo], W=[rmod])
        modv = modT[:].rearrange("p (j t) -> p j t", t=2)
        k.tt("dve", sh2[:, :, 0], modv[:, 0:8, 0], vT[:, 16:24], ALU.add, R=[rmod, rvT], W=[rsh2])
        k.tt("dve", gm[:], modv[:, 8:16, 0], vT[:, 24:32], ALU.add, R=[rmod, rvT], W=[rgm])
        k.ts("dve", gm[:], gm[:], 1.0, None, ALU.add, R=[rgm], W=[rgm])
        k.tt("dve", gm[:], gm[:], vT[:, 8:16], ALU.mult, R=[rgm, rvT], W=[rgm])
        for hp in range(8):
            W32, rW32 = W32_ring.next()
            k.dma(W32[:], win[hp].rearrange("(k p) n -> p k n", p=128), W=[rW32])
            for kk in range(8):
                if kk % 2 == 0:
                    k.ts("dve", Wall[:, kk, hp * 385:(hp + 1) * 385], W32[:, kk, :], gm[:, kk:kk + 1], None, ALU.mult,
                         R=[rW32, rgm], W=[rWall])
                else:
                    k.act(Wall[:, kk, hp * 385:(hp + 1) * 385], W32[:, kk, :], AF.Identity, scale=gm[:, kk:kk + 1],
                          R=[rW32, rgm], W=[rWall])
            k.cp("dve", Wf32[:, :, hp], W32[:, :, 384], R=[rW32], W=[rWf32])
            po, rpo = psO.next()
            for i, (c0, m) in enumerate(((64, 128), (256, 128), (0, 64), (192, 64))):
                for kk in range(8):
                    k.mm(po[0:m, 2 * i:2 * i + 2], W32[:, kk, c0:c0 + m], sh2[:, kk, :],
                         start=(kk == 0), stop=(kk == 7), R=[rW32, rsh2], W=[rpo])
            k.cp("dve", ball[:, hp * 8:(hp + 1) * 8], po[:, 0:8], R=[rpo], W=[rball])
        for kk in range(8):
            k.ts("dve", Wfb[:, kk, :], Wf32[:, kk, :], gm[:, kk:kk + 1], None, ALU.mult, R=[rWf32, rgm], W=[rWfb])
        po, rpo = psO.next()
        for kk in range(8):
            k.mm(po[0:8, 0:2], Wf32[:, kk, :], sh2[:, kk, :], start=(kk == 0), stop=(kk == 7),
                 R=[rWf32, rsh2], W=[rpo])
        k.tt("dve", negfb8[:, 0:1], po[0:8, 0:1], bf8[:, 0:1], ALU.add, R=[rpo, rbf8], W=[rnegfb])
        k.ts("dve", negfb8[:, 0:1], negfb8[:, 0:1], -1.0, None, ALU.mult, R=[rnegfb], W=[rnegfb])

        def load_x(g_):
            lst = []
            for i in range(4):
                t = 4 * g_ + i
                xt, rxt = xt_ring.next()
                k.dma(xt[:], x[t * 128:(t + 1) * 128, :], W=[rxt])
                lst.append((xt, rxt))
            return lst

        def make_xT_a(xts):
            for i in range(4):
                xt, rxt = xts[i]
                k.act(junk[:], xt[:], AF.Square, accum=ss[:, i:i + 1], R=[rxt], W=[rjunk, rss])
            k.act(rstd[:], ss[:], AF.Sqrt, bias=1e-6, scale=1.0 / D, R=[rss], W=[rrstd])
            k.recip(rstd[:], rstd[:], R=[rrstd], W=[rrstd])
            xns = []
            for i in range(4):
                xt, rxt = xts[i]
                xn, rxn = xn_ring.next()
                k.ts("dve", xn[:], xt[:], rstd[:, i:i + 1], None, ALU.mult, R=[rxt, rrstd], W=[rxn])
                xns.append((xn, rxn))
            return xns

        def make_xT_b(xns):
            xT, rxT = xT_ring.next()
            for i in range(4):
                xn, rxn = xns[i]
                pt, rpt = psT.next()
                for kk in range(8):
                    k.tr(pt[:, kk * 128:(kk + 1) * 128], xn[:, kk * 128:(kk + 1) * 128], idb[:],
                         R=[rxn, ridb], W=[rpt])
                k.cp("dve", xT[:, :, i * 128:(i + 1) * 128], pt[:].rearrange("p (k t) -> p k t", k=8),
                     R=[rpt], W=[rxT])
            return xT, rxT

        tmplG = {}
        tmplMB = {}
        rtmpl = Res()
        tmf = sb("tmplMBf", [128, 128])
        for g_ in OC:
            for own in (2 * g_, 2 * g_ + 1):
                tg = sb("tmplG%d" % own, [128, 64])
                k.memset("pool", tg[:], -1e30, W=[rtmpl])
                if own > 0:
                    k.cp("dve", tg[:, 0:own], padGs[:, 0:own], R=[rpadG, rtmpl], W=[rtmpl])
                tmplG[own] = tg
                tb = sb("tmplMB%d" % own, [128, 128], BF16)
                k.memset("pool", tmf[:, 0:64], 0.0, W=[rtmpl])
                k.memset("pool", tmf[:, 64:128], NEG, W=[rtmpl])
                k.memset("pool", tmf[:, 64 + own:65 + own], 0.0, W=[rtmpl])
                k.cp("dve", tb[:], tmf[:], R=[rtmpl], W=[rtmpl])
                tmplMB[own] = tb

        prev_c = None
        cur_xT = make_xT_b(make_xT_a(load_x(0)))
        pend_x = load_x(1) if NQC > 1 else None
        pend_xn = None
        for g in range(NQC):
            xT, rxT = cur_xT
            cols = slice(g * 512, (g + 1) * 512)
            own_g = g in OC
            oj = OC.index(g) if own_g else -1
            qcols = slice(oj * 512, (oj + 1) * 512)
            pf, rpf = psP.next()
            for kk in range(8):
                k.mm(pf[0:8, :], Wfb[:, kk, :], xT[:, kk, :], start=(kk == 0), stop=(kk == 7), R=[rWfb, rxT], W=[rpf])
            fr, rfr = fr_ring.next()
            k.act(fr[:, 0, :], pf[0:8, :], AF.Exp, bias=negfb8[:, 0:1], scale=-1.0, R=[rpf, rnegfb], W=[rfr])
            k.act(fr[:, 1, :], fr[:, 0, :], AF.Ln, bias=1.0, scale=1.0, R=[rfr], W=[rfr])
            cr, rcr = c_ring.next()
            init = 0.0 if prev_c is None else prev_c[0][:, 511:512]
            rd = [rfr, ronesf] + ([] if prev_c is None else [prev_c[1]])
            k.s.op("dve", lambda e, cr=cr, fr=fr, init=init: e.tensor_tensor_scan(
                cr[:, :], onesf[0:8, :], fr[:, 1, :], init, ALU.mult, ALU.subtract), rd, [rcr])
            k.dma(Cd8[:, cols], cr[:, :], R=[rcr], W=[rCd8], q="pool")
            if prev_c is None:
                k.cp("dve", fr[:, 2, :], cr[:, :], R=[rcr], W=[rfr])
            else:
                k.ts("dve", fr[:, 2, :], cr[:, :], init, None, ALU.subtract, R=[rcr, prev_c[1]], W=[rfr])
                k.cp("dve", crefrow8[:, g:g + 1], init, R=[prev_c[1]], W=[rcrefrow])
            if own_g:
                c3, rc3 = c3_ring.next()
                k.cp("dve", c3[:, 0, :], fr[:, 2, :], R=[rfr], W=[rc3])
                k.tt("dve", fr[:, 3, :], fr[:, 2, :], c3[:, 0, :], ALU.subtract, R=[rfr, rc3], W=[rfr])
                k.cp("dve", c3[:, 1, :], fr[:, 3, :], R=[rfr], W=[rc3])
                k.tt("dve", fr[:, 2, :], fr[:, 3, :], c3[:, 1, :], ALU.subtract, R=[rfr, rc3], W=[rfr])
                k.cp("dve", c3[:, 2, :], fr[:, 2, :], R=[rfr], W=[rc3])
                for r3 in range(3):
                    k.dma(C3d[:, r3, qcols], c3[:, r3, :], R=[rc3], W=[rC3d], q="pool")
            prev_c = (cr, rcr)
            for hp in range(8):
                for kind in range(2):
                    w0 = hp * 385
                    ko = w0 + (64 if kind == 0 else 256)
                    qo = w0 + (0 if kind == 0 else 192)
                    bki = hp * 8 + (0 if kind == 0 else 2)
                    bqi = hp * 8 + (4 if kind == 0 else 6)
                    pk, rpk = psP.next()
                    for kk in range(8):
                        k.mm(pk[:, :], Wall[:, kk, ko:ko + 128], xT[:, kk, :], start=(kk == 0), stop=(kk == 7),
                             R=[rWall, rxT], W=[rpk])
                    kst, rkst = kst_ring.next()
                    k.act(kst[:, :], pk[:, :], AF.Identity, bias=ball[:, bki:bki + 1], R=[rpk, rball], W=[rkst])
                    if kind == 1:
                        for hb in range(2):
                            k.act(junk[0:64, 0:256], pk[0:64, hb * 256:(hb + 1) * 256], AF.Copy,
                                  accum=km2[:, hb:hb + 1], R=[rpk], W=[rjunk, rkm2])
                        for hb in range(2):
                            c_ = hp * NBP + 2 * g + hb
                            k.act(kmall[:, c_:c_ + 1], km2[:, hb:hb + 1], AF.Identity,
                                  bias=ball[0:64, bki:bki + 1], scale=1.0 / 256, R=[rkm2, rball], W=[rkmean])
                    k.dma(KVd[hp, kind, :, cols], kst[:], R=[rkst], W=[rKVd[hp][kind]])
                    if kind == 1 and g + 1 < NQC:
                        if hp == 0:
                            pend_xn = make_xT_a(pend_x)
                            pend_x = load_x(g + 2) if g + 2 < NQC else None
                        elif hp == 3:
                            cur_xT = make_xT_b(pend_xn)
                    if not own_g:
                        continue
                    pq, rpq = psP.next()
                    for kk in range(8):
                        k.mm(pq[0:64, :], Wall[:, kk, qo:qo + 64], xT[:, kk, :], start=(kk == 0), stop=(kk == 7),
                             R=[rWall, rxT], W=[rpq])
                    qst, rqst = qst_ring.next()
                    k.ts("dve", qst[0:64, :], pq[0:64, :], ball[0:64, bqi:bqi + 1], 0.125, ALU.add, ALU.mult,
                         R=[rpq, rball], W=[rqst])
                    if kind == 0:
                        k.dma(Qd[hp, kind, 0:64, qcols], qst[0:64, :], R=[rqst], W=[rQd[hp][kind]])
                        continue
                    q32, rq32 = q32_ring.next()
                    k.act(q32[:], pq[0:64, :], AF.Identity, bias=ball[0:64, bqi:bqi + 1], R=[rpq, rball], W=[rq32])
                    for i in range(4):
                        own = 2 * g + i // 2
                        MB, rMB = MB_ring.next()
                        k.cp("pool", MB[:], tmplMB[own][:], R=[rtmpl], W=[rMB])
                        if own > 0:
                            pg, rpg = psO.next()
                            k.mm(pg[:, 0:NBP], q32[:, i * 128:(i + 1) * 128], kmall[:, hp * NBP:(hp + 1) * NBP],
                                 R=[rq32, rkmean], W=[rpg])
                            G, rG = G_ring.next()
                            k.tt("dve", G[:, 0:NBP], pg[:, 0:NBP], tmplG[own][:, 0:NBP], ALU.add, R=[rpg, rtmpl], W=[rG])
                            mx, rmx = mx_ring.next()
                            k.s.op("dve", lambda e, mx=mx, G=G: e.max(mx[:], G[:, 0:NBP]), [rG], [rmx])
                            k.ts("dve", MB[:, 64:64 + own], G[:, 0:own], mx[:, 2:3], NEG, ALU.is_lt, ALU.mult,
                                 R=[rG, rmx], W=[rMB])
                        pt, rpt = psT.next()
                        k.tr(pt[:, 0:128], MB[:], idb[:], R=[rMB, ridb], W=[rpt])
                        k.cp("dve", qst[64:128, i * 128:(i + 1) * 128], pt[64:128, 0:128], R=[rpt], W=[rqst])
                    k.dma(Qd[hp, kind, :, qcols], qst[:], R=[rqst], W=[rQd[hp][kind]])
        k.dma(Crd[:, :], crefrow8[:, :], R=[rcrefrow], W=[rCrd], q="pool")
        sch.flush()
        sW.close()

        sA = ExitStack()

        def sb(name, shape, dt=F32):
            return sA.enter_context(nc.sbuf_tensor(name, shape, dt))

        def pst(name, shape, dt=F32):
            return sA.enter_context(nc.psum_tensor(name, shape, dt))

        psT = Ring([pst("psT%d" % i, [128, 1024], BF16) for i in range(2)], x=True)
        psP = Ring([pst("psP%d" % i, [128, 512]) for i in range(4)], x=True)
        psO = Ring([pst("psO%d" % i, [128, 512]) for i in range(2)], x=True)
        Kts = [sb("Kt%d" % i, [128, S], BF16) for i in range(2)]
        Vts = [sb("Vt%d" % i, [128, NT, 65], BF16) for i in range(2)]
        Qts = [sb("Qt%d" % i, [128, TPC], BF16) for i in range(2)]
        rKs = [Res(), Res()]; rVs = [Res(), Res()]; rQs = [Res(), Res()]
        idf = sb("idf", [128, 128]); ridf = Res()
        idb = sb("idb", [128, 128], BF16); ridb = Res()
        onesf = sb("onesf", [128, 512]); ronesf = Res()
        Mfox = sb("Mfox", [128, 1024], BF16); rMfox = Res()
        Mm_ring = Ring([sb("Mmoba%d" % i, [128, 1024], BF16) for i in range(2)])
        mtmp = sb("mtmp", [128, 1024]); rmtmp = Res()
        mt2_ring = Ring([sb("mtmp2_%d" % i, [128, 1024]) for i in range(1)])
        rb31s = sb("rb31s", [128, 8]); rrb31 = Res()
        padks = sb("padks", [128, NT]); rpadk = Res()
        kbn = padks; rkbn = rpadk
        kbf_ring = Ring([sb("kbf%d" % i, [128, NT]) for i in range(2)])
        crow_ring = Ring([sb("crow%d" % i, [1, NC2]) for i in range(2)])
        crefB_ring = Ring([sb("crefB%d" % i, [128, NC2]) for i in range(2)])
        negC_ring = Ring([sb("negC%d" % i, [128, NT]) for i in range(2)])
        cdt_ring = Ring([sb("cdt%d" % i, [NT, 128]) for i in range(2)])
        vts_ring = Ring([sb("vts%d" % i, [128, 512], BF16) for i in range(3)])
        P_ring = Ring([sb("P%d" % i, [128, 512], BF16) for i in range(4)])
        ot_ring = Ring([sb("ot%d" % i, [128, 512]) for i in range(2)])
        yt_ring = Ring([sb("yt%d" % i, [64, 512], BF16) for i in range(2)])
        rl_ring = Ring([sb("rl%d" % i, [128, 512]) for i in range(1)])
        bq_ring = Ring([sb("bq%d" % i, [128, NT]) for i in range(2)])

        k.dma(idf[:], identf, W=[ridf])
        k.cp("dve", idb[:], idf[:], R=[ridf], W=[ridb])
        k.memset("pool", onesf[:], 1.0, W=[ronesf])
        k.dma(rb31s[:], rb31, W=[rrb31])
        k.dma(padks[:], padk, W=[rpadk])
        k.dma(mtmp[:], cmask, W=[rmtmp])
        k.cp("pool", Mfox[:], mtmp[:], R=[rmtmp], W=[rMfox])
        for par in range(2):
            k.memset("pool", Vts[par][:, :, 64:65], 1.0, W=[rVs[par]])

        def prep(p):
            hp, kind = p // 2, p % 2
            par = p % 2
            Kt, Vt, Qt = Kts[par], Vts[par], Qts[par]
            st_ = {"hp": hp, "kind": kind, "par": par}
            k.dma(Kt[0:64, :], KVd[hp, kind, 0:64, :], R=[rKVd[hp][kind]], W=[rKs[par]])
            if kind == 0:
                k.memset("pool", Kt[64:67, :], 1.0, W=[rKs[par]])
                k.dma(Qt[0:64, :], Qd[hp, kind, 0:64, :], R=[rQd[hp][kind]], W=[rQs[par]])
                k.dma(Qt[64:67, :], C3d[hp], R=[rC3d], W=[rQs[par]])
                crow, rcrow = crow_ring.next()
                k.dma(crow[:], Crd[hp:hp + 1, :], R=[rCrd], W=[rcrow])
                crefB, rcrefB = crefB_ring.next()
                po, rpo = psO.next()
                k.mm(po[:, 0:NC2], onesf[0:1, 0:128], crow[0:1, :], R=[ronesf, rcrow], W=[rpo])
                k.cp("dve", crefB[:], po[:, 0:NC2], R=[rpo], W=[rcrefB])
                cdt, rcdt = cdt_ring.next()
                k.dma(cdt[:], Cd8[hp].rearrange("(t p) -> t p", p=128), R=[rCd8], W=[rcdt])
                negC, rnegC = negC_ring.next()
                po, rpo = psO.next()
                k.tr(po[:, 0:NT], cdt[:], idf[0:NT, 0:NT], R=[rcdt, ridf], W=[rpo])
                k.ts("dve", negC[:], po[:, 0:NT], -1.0, None, ALU.mult, R=[rpo], W=[rnegC])
                k.tt("dve", negC[:], negC[:], padks[:], ALU.add, R=[rnegC, rpadk], W=[rnegC])
                st_.update(crefB=crefB, rcrefB=rcrefB, negC=negC, rnegC=rnegC, Mk=Mfox, rMk=rMfox)
            else:
                k.dma(Kt[64:128, :], onehot, W=[rKs[par]])
                k.dma(Qt[:, :], Qd[hp, kind], R=[rQd[hp][kind]], W=[rQs[par]])
                mt2, rmt2 = mt2_ring.next()
                k.dma(mt2[:], mbias[hp], W=[rmt2])
                Mm, rMm = Mm_ring.next()
                k.tt("pool", Mm[:], mtmp[:], mt2[:], ALU.add, R=[rmtmp, rmt2], W=[rMm])
                kbf, rkbf = kbf_ring.next()
                k.ts("dve", kbf[:], padks[:], rb31s[:, hp:hp + 1], None, ALU.add, R=[rpadk, rrb31], W=[rkbf])
                st_.update(kbf=kbf, rkbf=rkbf, Mk=Mm, rMk=rMm)
            def vgroup(g):
                vts, rvts = vts_ring.next()
                k.dma(vts[64:128, :], KVd[hp, kind, 64:128, g * 512:(g + 1) * 512], R=[rKVd[hp][kind]], W=[rvts])
                pv, rpv = psT.next()
                for i in range(4):
                    k.tr(pv[:, i * 64:(i + 1) * 64], vts[64:128, i * 128:(i + 1) * 128], idb[64:128, 64:128],
                         R=[rvts, ridb], W=[rpv])
                k.cp("dve", Vt[:, 4 * g:4 * g + 4, 0:64], pv[:, 0:256].rearrange("p (a b) -> p a b", a=4),
                     R=[rpv], W=[rVs[par]])
            st_["vthunks"] = [(lambda g=g: vgroup(g)) for g in range(NQC)]
            return st_

        def attn(st_, thunks):
            hp, kind, par = st_["hp"], st_["kind"], st_["par"]
            npairs = sum(4 * qc + 4 for qc in OC)
            stride = max(1, npairs // (len(thunks) + 1)) if thunks else 0
            cnt = [0]
            Kt, Vt, Qt = Kts[par], Vts[par], Qts[par]
            rK1, rV1, rQ1 = rKs[par], rVs[par], rQs[par]
            KR = 67 if kind == 0 else 128
            Mk, rMk = st_["Mk"], st_["rMk"]
            LA = 2
            pending = [None]

            def finalize(oj, po, rpo):
                t0 = oj * 512
                ot, rot = ot_ring.next()
                k.cp("dve", ot[0:65, :], po[0:65, :], R=[rpo], W=[rot])
                rl, rrl = rl_ring.next()
                k.recip(rl[64:65, :], ot[64:65, :], R=[rot], W=[rrl])
                pb, rpb = psP.next()
                k.mm(pb[0:64, :], onesf[64:65, 0:64], rl[64:65, :], R=[ronesf, rrl], W=[rpb])
                yt, ryt = yt_ring.next()
                k.tt("dve", yt[:], ot[0:64, :], pb[0:64, :], ALU.mult, R=[rot, rpb], W=[ryt])
                k.dma(mixd[hp, kind * 64:(kind + 1) * 64, t0:t0 + 512], yt[:], R=[ryt], W=[rMix], q="pool")

            for oj, qc in enumerate(OC):
                t0 = oj * 512
                nk = 4 * qc + 4
                if kind == 0:
                    bq, rbq = bq_ring.next()
                    k.ts("dve", bq[:, 0:nk], st_["negC"][:, 0:nk], st_["crefB"][:, qc:qc + 1], None, ALU.add,
                         R=[st_["rnegC"], st_["rcrefB"]], W=[rbq])
                po, rpo = psO.next()
                Ps = {}
                for it in range(nk + LA):
                    kt = it
                    if kt < nk:
                        j = kt - 4 * qc
                        near = (j >= 0) if kind == 0 else (j >= -1)
                        pS, rpS = psP.next()
                        k.mm(pS[:], Kt[0:KR, kt * 128:(kt + 1) * 128], Qt[0:KR, t0:t0 + 512], start=True,
                             stop=not near, R=[rK1, rQ1], W=[rpS])
                        if near:
                            s0_ = 384 - 128 * j
                            k.mm(pS[:], idb[:], Mk[:, s0_:s0_ + 512], start=False, stop=True, R=[ridb, rMk], W=[rpS])
                        P, rP = P_ring.next()
                        if kind == 0:
                            k.act(P[:], pS[:], AF.Exp, bias=bq[:, kt:kt + 1], R=[rpS, rbq], W=[rP])
                        elif near:
                            k.act(P[:], pS[:], AF.Exp, bias=kbn[:, kt:kt + 1], R=[rpS, rkbn], W=[rP])
                        else:
                            k.act(P[:], pS[:], AF.Exp, bias=st_["kbf"][:, kt:kt + 1], R=[rpS, st_["rkbf"]], W=[rP])
                        Ps[kt] = (P, rP)
                    if it == LA and pending[0] is not None:
                        finalize(*pending[0])
                        pending[0] = None
                    kv = it - LA
                    if kv >= 0:
                        P, rP = Ps.pop(kv)
                        k.mm(po[0:65, :], Vt[:, kv, 0:65], P[:], start=(kv == 0), stop=(kv == nk - 1),
                             R=[rV1, rP], W=[rpo])
                        cnt[0] += 1
                        if thunks and cnt[0] % stride == 0:
                            thunks.pop(0)()
                if pending[0] is not None:
                    finalize(*pending[0])
                pending[0] = (oj, po, rpo)
            if pending[0] is not None:
                finalize(*pending[0])
                pending[0] = None
            while thunks:
                thunks.pop(0)()

        nxt = prep(0)
        for th in nxt["vthunks"]:
            th()
        for p in range(16):
            cur = nxt
            thunks = []
            if p + 1 < 16:
                nxt = prep(p + 1)
                thunks = list(nxt["vthunks"])
            attn(cur, thunks)
        sch.flush()
        sA.close()

        sB = ExitStack()

        def mk_alloc(stack, prefix):
            def sb_(name, shape, dt=F32):
                return stack.enter_context(nc.sbuf_tensor(prefix + name, shape, dt))
            return sb_
        sb = mk_alloc(sB, "B_")

        def pst(name, shape, dt=F32):
            return sB.enter_context(nc.psum_tensor("B_" + name, shape, dt))

        psA = Ring([pst("psA%d" % i, [128, 512]) for i in range(8)], x=True)
        idf = sb("idf", [128, 128]); ridf = Res()
        onesf = sb("onesf", [128, 128]); ronesf = Res()
        onesb = sb("onesb", [128, 128], BF16); ronesb = Res()
        h2Tb = sb("h2Tb", [128, 8, TPC], BF16); rh2b = [Res() for _ in range(NTL)]
        Gt = sb("Gts", [128, NTL, NE]); rGt = Res()
        GF = sb("GF", [128, D]); rGF = Res()
        bguT = sb("bguT", [128, 16 * NE]); rbguT = Res()
        rX1 = Res()
        rOut = Res()
        k.dma(idf[:], identf, W=[ridf])
        k.memset("pool", onesf[:], 1.0, W=[ronesf])
        k.memset("pool", onesb[:], 1.0, W=[ronesb])

        with ExitStack() as s1:
            sb1 = mk_alloc(s1, "B1_")
            vin = sb1("vin", [64, 128]); rvin = Res()
            vT = sb1("vT", [128, 64]); rvT = Res()
            cond2 = sb1("cond2", [128, 8, 2]); rcond = Res()
            modT = sb1("modT", [128, 64]); rmod = Res()
            modf = sb1("modf", [128, 32]); rmodf = Res()
            v8 = sb1("v8", [128, 4, 8]); rv8 = Res()
            dg = sb1("dg", [128, 8, 128]); rdg = Res()
            GG = sb1("GG", [128, D]); rGG = Res()
            G2 = sb1("G2", [128, D]); rG2 = Res()
            SH2 = sb1("SH2", [128, D]); rSH2 = Res()
            bgus = sb1("bgus", [NE, 2 * D]); rbgus = Res()
            wr = sb1("wrs", [128, 8, NE]); rwr = Res()
            brs = sb1("brs", [1, NE]); rbrs = Res()
            mixT = sb1("mixTs", [128, 8, TPC], BF16); rmix = Res()
            wob = sb1("wob", [128, 8, D], BF16); rwob = Res()
            L = sb1("L", [128, NTL, NE]); rL = Res()
            Self_ = sb1("Self", [128, NTL, NE]); rSelf = Res()
            junkb = sb1("junkb", [128, D], BF16); rjunk = Res()
            ssA = sb1("ssA", [128, 4]); rssA = Res()
            st_ring = Ring([sb1("stg%d" % i, [128, D]) for i in range(2)])
            wa_ring = Ring([sb1("wa%d" % i, [128, 8, 512]) for i in range(2)])
            xt_ring = Ring([sb1("xt%d" % i, [128, D]) for i in range(3)])
            x1_ring = Ring([sb1("x1%d" % i, [128, D]) for i in range(2)])
            h2f_ring = Ring([sb1("h2f%d" % i, [128, D]) for i in range(2)])
            h2T_ring = Ring([sb1("h2T%d" % i, [128, 8, 128]) for i in range(2)])
            sm_ring = Ring([sb1("sm%d" % i, [128, 8 + 4 * NE + 8]) for i in range(2)])

            k.dma(vin[:], vecsB, W=[rvin])
            k.dma(bgus[:], bgu_d, W=[rbgus])
            k.dma(wr[:], wr_d.rearrange("(k p) n -> p k n", p=128), W=[rwr])
            k.dma(brs[:], br_d, W=[rbrs])
            for r in range(8):
                k.dma(mixT[:, r, :], mixd[r], R=[rMix], W=[rmix])
            for r in range(8):
                sg, rsg = st_ring.next()
                k.dma(sg[:], wout[r], W=[rsg])
                k.cp("pool", wob[:, r, :], sg[:], R=[rsg], W=[rwob])
            po, rpo = psA.next()
            k.tr(po[:, 0:64], vin[:], idf[0:64, 0:64], R=[rvin, ridf], W=[rpo])
            k.cp("dve", vT[:], po[:, 0:64], R=[rpo], W=[rvT])
            k.memset("pool", cond2[:], 0.0, W=[rcond])
            k.act(cond2[:, :, 0], vT[:, 0:8], AF.Silu, R=[rvT], W=[rcond])
            po, rpo = psA.next()
            for blk in range(8):
                wa, rwa = wa_ring.next()
                k.dma(wa[:], wadaB[:, blk * 512:(blk + 1) * 512].rearrange("(k p) n -> p k n", p=128), W=[rwa])
                for jj in range(4):
                    j = blk * 4 + jj
                    for kk in range(8):
                        k.mm(po[:, 2 * j:2 * j + 2], wa[:, kk, jj * 128:(jj + 1) * 128], cond2[:, kk, :],
                             start=(kk == 0), stop=(kk == 7), R=[rwa, rcond], W=[rpo])
            k.cp("dve", modT[:], po[:, 0:64], R=[rpo], W=[rmod])
            modv = modT[:].rearrange("p (j t) -> p j t", t=2)
            k.tt("dve", modf[:], modv[:, :, 0], vT[:, 8:40], ALU.add, R=[rmod, rvT], W=[rmodf])
            k.tt("dve", v8[:, 0, :], modf[:, 0:8], vT[:, 40:48], ALU.mult, R=[rmodf, rvT], W=[rv8])
            k.ts("dve", v8[:, 1, :], modf[:, 16:24], 1.0, None, ALU.add, R=[rmodf], W=[rv8])
            k.tt("dve", v8[:, 1, :], v8[:, 1, :], vT[:, 48:56], ALU.mult, R=[rv8, rvT], W=[rv8])
            k.cp("dve", v8[:, 2, :], modf[:, 8:16], R=[rmodf], W=[rv8])
            k.tt("dve", v8[:, 3, :], modf[:, 24:32], vT[:, 56:64], ALU.mult, R=[rmodf, rvT], W=[rv8])
            for vi, (dst, rdst) in enumerate(((GG, rGG), (G2, rG2), (SH2, rSH2), (GF, rGF))):
                for kk in range(8):
                    k.ts("dve", dg[:, kk, :], idf[:], v8[:, vi, kk:kk + 1], None, ALU.mult, R=[ridf, rv8], W=[rdg])
                for half in range(2):
                    po, rpo = psA.next()
                    k.mm(po[:], onesf[:], dg[:, 4 * half:4 * half + 4, :], R=[ronesf, rdg], W=[rpo])
                    k.cp("act", dst[:, half * 512:(half + 1) * 512], po[:], R=[rpo], W=[rdst])
            po, rpo = psA.next()
            for fc in range(16):
                k.tr(po[:, fc * NE:(fc + 1) * NE], bgus[:, fc * 128:(fc + 1) * 128], idf[0:NE, 0:NE],
                     R=[rbgus, ridf], W=[rpo])
            k.cp("dve", bguT[:], po[:], R=[rpo], W=[rbguT])

            for i in range(NTL):
                tc = slice(i * 128, (i + 1) * 128)
                xt, rxt = xt_ring.next()
                k.dma(xt[:], x[xrow(i):xrow(i) + 128, :], W=[rxt])
                pp = []
                for half in range(2):
                    po, rpo = psA.next()
                    for r in range(8):
                        k.mm(po[:], mixT[:, r, tc], wob[:, r, half * 512:(half + 1) * 512],
                             start=(r == 0), stop=(r == 7), R=[rmix, rwob], W=[rpo])
                    k.act(junkb[:, 0:512], po[:], AF.Square, accum=ssA[:, half:half + 1], R=[rpo], W=[rjunk, rssA])
                    pp.append((po, rpo))
                sm, rsm = sm_ring.next()
                k.tt("dve", sm[:, 0:1], ssA[:, 0:1], ssA[:, 1:2], ALU.add, R=[rssA], W=[rsm])
                k.act(sm[:, 1:2], sm[:, 0:1], AF.Sqrt, bias=1e-6, scale=1.0 / D, R=[rsm], W=[rsm])
                k.recip(sm[:, 2:3], sm[:, 1:2], R=[rsm], W=[rsm])
                x1, rx1 = x1_ring.next()
                for half in range(2):
                    po, rpo = pp[half]
                    hs = slice(half * 512, (half + 1) * 512)
                    k.stt(x1[:, hs], po[:], sm[:, 2:3], GG[:, hs], ALU.mult, ALU.mult, R=[rpo, rsm, rGG], W=[rx1])
                k.tt("dve", x1[:], x1[:], xt[:], ALU.add, R=[rx1, rxt], W=[rx1])
                k.dma(X1d[tc, :], x1[:], R=[rx1], W=[rX1], q="pool")
                k.act(junkb[:], x1[:], AF.Square, accum=ssA[:, 2:3], R=[rx1], W=[rjunk, rssA])
                k.act(sm[:, 3:4], ssA[:, 2:3], AF.Sqrt, bias=1e-6, scale=1.0 / D, R=[rssA], W=[rsm])
                k.recip(sm[:, 4:5], sm[:, 3:4], R=[rsm], W=[rsm])
                h2f, rh2f = h2f_ring.next()
                k.stt(h2f[:], x1[:], sm[:, 4:5], G2[:], ALU.mult, ALU.mult, R=[rx1, rsm, rG2], W=[rh2f])
                k.tt("dve", h2f[:], h2f[:], SH2[:], ALU.add, R=[rh2f, rSH2], W=[rh2f])
                h2T, rh2T = h2T_ring.next()
                for half in range(2):
                    po, rpo = psA.next()
                    for kk in range(4):
                        c0 = (half * 4 + kk) * 128
                        k.tr(po[:, kk * 128:(kk + 1) * 128], h2f[:, c0:c0 + 128], idf[:], R=[rh2f, ridf], W=[rpo])
                    k.cp("act", h2T[:, half * 4:half * 4 + 4, :], po[:].rearrange("p (k t) -> p k t", k=4),
                         R=[rpo], W=[rh2T])
                    k.cp("dve", h2Tb[:, half * 4:half * 4 + 4, tc], po[:].rearrange("p (k t) -> p k t", k=4),
                         R=[rpo], W=[rh2b[i]])
                po, rpo = psA.next()
                for kk in range(8):
                    k.mm(po[:, 0:NE], h2T[:, kk, :], wr[:, kk, :], start=(kk == 0), stop=False, R=[rh2T, rwr], W=[rpo])
                k.mm(po[:, 0:NE], onesf[0:1, :], brs[0:1, :], start=False, stop=True, R=[ronesf, rbrs], W=[rpo])
                k.cp("dve", L[:, i, :], po[:, 0:NE], R=[rpo], W=[rL])
                mx = sm[:, 8:16]
                k.s.op("dve", lambda e, mx=mx, Li=L[:, i, :]: e.max(mx, Li), [rL], [rsm])
                k.ts("dve", Self_[:, i, :], L[:, i, :], mx[:, 3:4], None, ALU.is_ge, R=[rL, rsm], W=[rSelf])
                k.ts("dve", sm[:, 5:6], mx[:, 0:1], -1.0, None, ALU.mult, R=[rsm], W=[rsm])
                ex = sm[:, 16:16 + NE]
                ew = sm[:, 16 + NE:16 + 2 * NE]
                k.act(ex, L[:, i, :], AF.Exp, bias=sm[:, 5:6], R=[rL, rsm], W=[rsm])
                k.tt("dve", ew, ex, Self_[:, i, :], ALU.mult, R=[rsm, rSelf], W=[rsm])
                k.s.op("dve", lambda e, z=sm[:, 6:7], ew=ew: e.tensor_reduce(z, ew, AX.X, ALU.add), [rsm], [rsm])
                k.recip(sm[:, 7:8], sm[:, 6:7], R=[rsm], W=[rsm])
                k.ts("dve", Gt[:, i, :], ew, sm[:, 7:8], None, ALU.mult, R=[rsm], W=[rGt])
            sch.flush()

        with ExitStack() as s2:
            sb2 = mk_alloc(s2, "B2_")
            CH = min(512, TPC)
            NCH = TPC // CH
            yacc = sb2("yacc", [128, NTL, D]); ryacc = [Res() for _ in range(NTL)]
            actT = sb2("actT", [128, 8, TPC], BF16); ract = [Res() for _ in range(NCH)]
            bdall = sb2("bdall", [NE, D]); rbdall = Res()
            gtT_ring = Ring([sb2("gtT%d" % i, [NE, 128]) for i in range(2)])
            w_ring = Ring([sb2("wu%d" % i, [128, 8, 512], BF16) for i in range(4)])
            sg_ring = Ring([sb2("sg%d" % i, [128, 512]) for i in range(4)])
            xg_t = sb2("xg", [128, CH]); rxg = Res()
            sig_t = sb2("sig", [128, CH]); rsig = Res()
            xl_t = sb2("xl", [128, CH]); rxl = Res()
            xt_ring = Ring([sb2("xf%d" % i, [128, D]) for i in range(2)])
            sm2 = Ring([sb2("sq%d" % i, [128, 4]) for i in range(2)])
            junk2 = sb2("junk2", [128, D], BF16); rjunk2 = Res()

            k.memset("pool", yacc[:], 0.0, W=ryacc)
            k.dma(bdall[:], bd_d, W=[rbdall])

            def load_unit_q(e, q):
                w, rw = w_ring.next()
                for kk in range(8):
                    sg, rsg = sg_ring.next()
                    src = wgu_d[e, kk * 128:(kk + 1) * 128, :].rearrange("p (two c) -> p two c", two=2)
                    k.dma(sg[:].rearrange("p (two c) -> p two c", two=2), src[:, :, q * 256:(q + 1) * 256], W=[rsg])
                    k.cp("pool", w[:, kk, :], sg[:], R=[rsg], W=[rw])
                return w, rw

            def load_unit_d(e, nh):
                w, rw = w_ring.next()
                for fk in range(8):
                    sg, rsg = sg_ring.next()
                    k.dma(sg[:], wd_d[e, fk * 128:(fk + 1) * 128, nh * 512:(nh + 1) * 512], W=[rsg])
                    k.cp("pool", w[:, fk, :], sg[:], R=[rsg], W=[rw])
                return w, rw

            for e in range(NE):
                for q in range(4):
                    w, rw = load_unit_q(e, q)
                    for ch in range(NCH):
                        cs = slice(ch * CH, (ch + 1) * CH)
                        for f2 in range(2):
                            fcl = q * 2 + f2
                            pgt, rpgt = psA.next()
                            for kk in range(8):
                                k.mm(pgt[:, 0:CH], w[:, kk, f2 * 128:(f2 + 1) * 128], h2Tb[:, kk, cs],
                                     start=(kk == 0), stop=(kk == 7), R=[rw] + rh2b, W=[rpgt])
                            plt, rplt = psA.next()
                            for kk in range(8):
                                k.mm(plt[:, 0:CH], w[:, kk, 256 + f2 * 128:256 + (f2 + 1) * 128], h2Tb[:, kk, cs],
                                     start=(kk == 0), stop=(kk == 7), R=[rw] + rh2b, W=[rplt])
                            bg = bguT[:, fcl * NE + e:fcl * NE + e + 1]
                            bl = bguT[:, (8 + fcl) * NE + e:(8 + fcl) * NE + e + 1]
                            k.ts("dve", xg_t[:], pgt[:, 0:CH], bg, 7.0, ALU.add, ALU.min, R=[rpgt, rbguT], W=[rxg])
                            k.act(sig_t[:], xg_t[:], AF.Sigmoid, scale=1.702, R=[rxg], W=[rsig])
                            k.ts("dve", xl_t[:], plt[:, 0:CH], bl, 7.0, ALU.add, ALU.min, R=[rplt, rbguT], W=[rxl])
                            k.ts("dve", xl_t[:], xl_t[:], -7.0, 1.0, ALU.max, ALU.add, R=[rxl], W=[rxl])
                            k.tt("dve", xg_t[:], xg_t[:], sig_t[:], ALU.mult, R=[rxg, rsig], W=[rxg])
                            k.tt("dve", actT[:, fcl, cs], xg_t[:], xl_t[:], ALU.mult, R=[rxg, rxl], W=[ract[ch]])
                for nh in range(2):
                    w, rw = load_unit_d(e, nh)
                    for i in range(NTL):
                        tc = slice(i * 128, (i + 1) * 128)
                        py, rpy = psA.next()
                        for fk in range(8):
                            k.mm(py[:], actT[:, fk, tc], w[:, fk, :], start=(fk == 0), stop=(fk == 7),
                                 R=[ract[(i * 128) // CH], rw], W=[rpy])
                        ys = yacc[:, i, nh * 512:(nh + 1) * 512]
                        k.stt(ys, py[:], Gt[:, i, e:e + 1], ys, ALU.mult, ALU.add, R=[rpy, rGt, ryacc[i]], W=[ryacc[i]])
            for i in range(NTL):
                tc = slice(i * 128, (i + 1) * 128)
                xt, rxt = xt_ring.next()
                k.dma(xt[:], X1d[tc, :], R=[rX1], W=[rxt])
                pg_, rpg_ = psA.next()
                k.tr(pg_[0:NE, 0:128], Gt[:, i, :], idf[:], R=[rGt, ridf], W=[rpg_])
                gtT, rgtT = gtT_ring.next()
                k.cp("dve", gtT[:], pg_[0:NE, 0:128], R=[rpg_], W=[rgtT])
                for nh in range(2):
                    pb_, rpb_ = psA.next()
                    k.mm(pb_[:], gtT[:], bdall[:, nh * 512:(nh + 1) * 512], R=[rgtT, rbdall], W=[rpb_])
                    ys = yacc[:, i, nh * 512:(nh + 1) * 512]
                    k.tt("dve", ys, ys, pb_[:], ALU.add, R=[ryacc[i], rpb_], W=[ryacc[i]])
                sq, rsq = sm2.next()
                k.act(junk2[:], yacc[:, i, :], AF.Square, accum=sq[:, 0:1], R=[ryacc[i]], W=[rjunk2, rsq])
                k.act(sq[:, 1:2], sq[:, 0:1], AF.Sqrt, bias=1e-6, scale=1.0 / D, R=[rsq], W=[rsq])
                k.recip(sq[:, 2:3], sq[:, 1:2], R=[rsq], W=[rsq])
                k.stt(yacc[:, i, :], yacc[:, i, :], sq[:, 2:3], GF[:], ALU.mult, ALU.mult, R=[ryacc[i], rsq, rGF], W=[ryacc[i]])
                k.tt("dve", yacc[:, i, :], yacc[:, i, :], xt[:], ALU.add, R=[ryacc[i], rxt], W=[ryacc[i]])
                k.dma(out[tc, :], yacc[:, i, :], R=[ryacc[i]], W=[rOut])
            sch.flush(final=[rOut])
        sB.close()
    return nc


def t5_bucket_np(dist):
    dist = np.maximum(dist, 0)
    max_exact = 16
    d = np.maximum(dist, 1).astype(np.float32)
    large = max_exact + (np.log(d / np.float32(max_exact)) / np.float32(np.log(128 / 16)) * np.float32(16)).astype(np.int32)
    large = np.minimum(large, 31)
    return np.where(dist < max_exact, dist, large)


def fused_inputs(S, inp):
    x = inp["x"].reshape(S, D)
    NT = S // 128
    W = inp["w_in"][0]
    win = np.stack([np.ascontiguousarray(W[:, np.concatenate([
        np.arange(h * 64, h * 64 + 64), 512 + np.arange(h * 64, h * 64 + 64),
        1024 + np.arange(h * 64, h * 64 + 64), 1536 + np.arange(h * 64, h * 64 + 64),
        2048 + np.arange(h * 64, h * 64 + 64), 2560 + np.arange(h * 64, h * 64 + 64),
        np.array([3072 + h])])]) for h in range(8)], axis=0)
    w_ada = inp["w_ada"][0]
    ba = inp["b_ada"][0]
    c = inp["c"]
    vecsA = np.concatenate([c.reshape(8, 128), inp["g_pre_mix"][0].reshape(8, 128), ba[0:1024].reshape(8, 128),
                            ba[1024:2048].reshape(8, 128)], axis=0).astype(np.float32)
    vecsB = np.concatenate([c.reshape(8, 128), ba[2048:6144].reshape(32, 128), inp["g_post_mix"][0].reshape(8, 128),
                            inp["g_pre_ffn"][0].reshape(8, 128), inp["g_post_ffn"][0].reshape(8, 128)],
                           axis=0).astype(np.float32)
    p = np.arange(128)[:, None]
    xx = np.arange(1024)[None, :]
    delta = xx - 384 - p
    cmask = np.where(delta >= 0, 0.0, NEG).astype(np.float32)
    bucket = t5_bucket_np(delta)
    rel_bias = inp["rel_bias"]
    mbias = np.stack([rel_bias[bucket, h] for h in range(8)], axis=0).astype(np.float32)
    rb31 = np.tile(rel_bias[31, :][None, :], (128, 1)).astype(np.float32)
    oh = np.zeros((64, S), dtype=ml_dtypes.bfloat16)
    for j in range(min(S // 256, 64)):
        oh[j, j * 256:(j + 1) * 256] = 1.0
    Wo = inp["w_out"][0]
    wout = np.stack([np.concatenate([Wo[r * 64:(r + 1) * 64], Wo[512 + r * 64:512 + (r + 1) * 64]], axis=0)
                     for r in range(8)], axis=0)
    shared = {"win": win, "wadaA": np.ascontiguousarray(w_ada[:, 0:2048]), "vecsA": vecsA,
              "bfg": inp["b_forget"][0].reshape(1, 8).astype(np.float32), "mbias": mbias, "cmask": cmask,
              "rb31": rb31, "onehot": oh, "identf": np.eye(128, dtype=np.float32),
              "wout": wout, "wadaB": np.ascontiguousarray(w_ada[:, 2048:6144]), "vecsB": vecsB,
              "wr": inp["w_router"][0], "br": inp["b_router"][0].reshape(1, NE),
              "wgu": inp["w_gate_up"][0], "bgu": inp["b_gate_up"][0], "wd": inp["w_down"][0], "bd": inp["b_down"][0]}
    maps = []
    for cix in range(NCORES):
        pad = 512 * (7 - cix)
        xw = np.zeros((S, D), dtype=np.float32)
        xw[pad:] = x[0:S - pad]
        padk = np.zeros((128, NT), dtype=np.float32)
        padk[:, 0:pad // 128] = NEG
        padG = np.zeros((128, 64), dtype=np.float32)
        padG[:, 0:pad // 256] = -1e30
        m = dict(shared)
        m.update({"x": xw, "padk": padk, "padG": padG})
        maps.append(m)
    return maps


_CACHE = {}


def run_fused(inp, dbg=False):
    S = inp["x"].shape[1]
    key = ("f", S, dbg)
    if key not in _CACHE:
        _CACHE[key] = build_fused(S, dbg=dbg)
    nc = _CACHE[key]
    maps = fused_inputs(S, inp)
    res = run_bass_kernel_spmd(nc, maps, core_ids=list(range(NCORES)))
    NOC = (S // 512) // 8
    out = np.zeros((S, D), dtype=np.float32)
    for cix in range(NCORES):
        o = np.asarray(res.results[cix]["out"])
        for j in range(NOC):
            rc = cix + 8 * j
            out[rc * 512:(rc + 1) * 512] = o[j * 512:(j + 1) * 512]
    return out, res


def kernel(**inputs):
    inp = {k_: np.asarray(v) for k_, v in inputs.items()}
    S = inp["x"].shape[1]
    out, _ = run_fused(inp)
    return out.reshape(1, S, D).astype(np.float32)
```
